# Optimizing a Trainium2 kernel written in Bass

```python
import math
import jax
import jax.numpy as jnp
from jax import lax
import numpy as np

D_MODEL = 4096
BATCH = 1
SEQ = 16384
DEPTH = 2

CHUNK = 64
D_MIX = D_MODEL
W_A = D_MIX // 4
A_HEAD_DIM = 128
A_HEADS = W_A // A_HEAD_DIM
W_B = D_MIX // 4
S5_GROUP = 16
S5_GROUPS = W_B // S5_GROUP
S5_STATE = 64
W_C = D_MIX // 4
C_HEADS = 4
C_HEAD_DIM = W_C // C_HEADS
ROPE_THETA = 10000.0
W_D = D_MIX - W_A - W_B - W_C
D_BLOCKS = 8
D_BLOCK = W_D // D_BLOCKS
CONV_WIDTH = 4
RG_C = 8.0
D_FF = 14336
N_EXPERTS = 8
TOP_K = 2
D_FF_EXPERT = 4096
N_DENSE = (DEPTH + 1) // 2
N_MOE = DEPTH // 2
EPS = 1e-6
IN_SPLITS = (W_A, W_A, W_A, W_A, W_B, W_C, W_C, W_C, W_C, W_D, W_D)
D_IN = 4 * W_A + W_B + 4 * W_C + 2 * W_D

kernel_name = 'hybrid_parallel_mixer_block'


def rms_norm(x, g):
    xf = x.astype(jnp.float32)
    y = xf * lax.rsqrt(jnp.mean(xf * xf, axis=-1, keepdims=True) + EPS)
    return (y * g.astype(jnp.float32)).astype(x.dtype)


def to_chunks(t):
    b_, s_ = t.shape[:2]
    return jnp.moveaxis(t.reshape(b_, s_ // CHUNK, CHUNK, *t.shape[2:]), 1, 0)


def from_chunks(t):
    nc, b_, l_ = t.shape[:3]
    return jnp.moveaxis(t, 0, 1).reshape(b_, nc * l_, *t.shape[3:])


def hgrn2_mixer(zq, zf, zi, zg, lb, norm_g):
    b_, s_, _ = zq.shape
    hd = (b_, s_, A_HEADS, A_HEAD_DIM)
    f32 = jnp.float32
    q = jax.nn.silu(zq.astype(f32)).reshape(hd)
    f = lb + (1.0 - lb) * jax.nn.sigmoid(zf.astype(f32))
    log_f = jnp.log(f).reshape(hd)
    k = (1.0 - f).reshape(hd)
    v = zi.astype(f32).reshape(hd)
    causal = jnp.tril(jnp.ones((CHUNK, CHUNK), dtype=bool))[None, :, :, None, None]

    def step(state, inp):
        qc, kc, vc, lfc = inp
        cum = jnp.cumsum(lfc, axis=1)
        rel = jnp.where(causal, cum[:, :, None] - cum[:, None, :], -jnp.inf)
        scores = jnp.einsum('bthd,bshd,btshd->bhts', qc, kc, jnp.exp(rel))
        o = (jnp.einsum('bhts,bshv->bthv', scores, vc)
             + jnp.einsum('bthd,bhdv->bthv', qc * jnp.exp(cum), state))
        last = cum[:, -1]
        state = (jnp.exp(last)[..., None] * state
                 + jnp.einsum('bshd,bshv->bhdv', kc * jnp.exp(last[:, None] - cum), vc))
        return state, o

    state0 = jnp.zeros((b_, A_HEADS, A_HEAD_DIM, A_HEAD_DIM), f32)
    _, o = lax.scan(step, state0, (to_chunks(q), to_chunks(k), to_chunks(v), to_chunks(log_f)))
    o = from_chunks(o)
    o = o * lax.rsqrt(jnp.mean(o * o, axis=-1, keepdims=True) + EPS)
    return o.reshape(b_, s_, W_A) * norm_g.astype(f32) * jax.nn.silu(zg.astype(f32))


def _complex_affine_combine(e1, e2):
    a1r, a1i, b1r, b1i = e1
    a2r, a2i, b2r, b2i = e2
    return (a2r * a1r - a2i * a1i,
            a2r * a1i + a2i * a1r,
            a2r * b1r - a2i * b1i + b2r,
            a2r * b1i + a2i * b1r + b2i)


def s5_mixer(u, lam_re, lam_im, log_dt, b_re, b_im, c_re, c_im, d_skip, glu_w, glu_b):
    f32 = jnp.float32
    b_, s_, _ = u.shape
    uf = u.astype(f32)
    lam_re = lam_re.astype(f32)
    lam_im = lam_im.astype(f32)
    dt = jnp.exp(log_dt.astype(f32))[:, None]
    mag = jnp.exp(lam_re * dt)
    ang = lam_im * dt
    ab_re = mag * jnp.cos(ang)
    ab_im = mag * jnp.sin(ang)
    den = lam_re * lam_re + lam_im * lam_im
    num_re = ab_re - 1.0
    coef_re = (num_re * lam_re + ab_im * lam_im) / den
    coef_im = (ab_im * lam_re - num_re * lam_im) / den
    br = b_re.astype(f32)
    bi = b_im.astype(f32)
    bb_re = coef_re[..., None] * br - coef_im[..., None] * bi
    bb_im = coef_re[..., None] * bi + coef_im[..., None] * br
    cr = c_re.astype(f32)
    ci = c_im.astype(f32)
    ug = uf.reshape(b_, s_, S5_GROUPS, S5_GROUP)

    def step(carry, uc):
        s_re, s_im = carry
        bu_re = jnp.einsum('blgp,gnp->blgn', uc, bb_re)
        bu_im = jnp.einsum('blgp,gnp->blgn', uc, bb_im)
        bu_re = bu_re.at[:, 0].add(ab_re * s_re - ab_im * s_im)
        bu_im = bu_im.at[:, 0].add(ab_re * s_im + ab_im * s_re)
        a_re = jnp.broadcast_to(ab_re, bu_re.shape)
        a_im = jnp.broadcast_to(ab_im, bu_im.shape)
        _, _, x_re, x_im = lax.associative_scan(_complex_affine_combine, (a_re, a_im, bu_re, bu_im), axis=1)
        y = jnp.einsum('blgn,gpn->blgp', x_re, cr) - jnp.einsum('blgn,gpn->blgp', x_im, ci)
        return (x_re[:, -1], x_im[:, -1]), y

    carry0 = (jnp.zeros((b_, S5_GROUPS, S5_STATE), f32), jnp.zeros((b_, S5_GROUPS, S5_STATE), f32))
    _, y = lax.scan(step, carry0, to_chunks(ug))
    y = from_chunks(y).reshape(b_, s_, W_B) + d_skip.astype(f32) * uf
    y = jax.nn.gelu(y)
    return y * jax.nn.sigmoid(y @ glu_w.astype(f32) + glu_b.astype(f32))


def rope(t, cos, sin):
    half = t.shape[-1] // 2
    t1, t2 = t[..., :half], t[..., half:]
    return jnp.concatenate([t1 * cos - t2 * sin, t1 * sin + t2 * cos], axis=-1)


def retention_mixer(zq, zk, zv, zg, norm_g):
    f32 = jnp.float32
    b_, s_, _ = zq.shape
    hd = (b_, s_, C_HEADS, C_HEAD_DIM)
    pos = jnp.arange(s_, dtype=f32)
    inv_freq = ROPE_THETA ** (-jnp.arange(0, C_HEAD_DIM, 2, dtype=f32) / C_HEAD_DIM)
    ang = pos[:, None] * inv_freq[None, :]
    cos = jnp.cos(ang)[None, :, None, :]
    sin = jnp.sin(ang)[None, :, None, :]
    q = rope(zq.astype(f32).reshape(hd), cos, sin)
    k = rope(zk.astype(f32).reshape(hd), cos, sin) * (C_HEAD_DIM ** -0.5)
    v = zv.astype(f32).reshape(hd)
    log_gamma = jnp.log(1.0 - 2.0 ** (-5.0 - jnp.arange(C_HEADS, dtype=f32)))
    idx = jnp.arange(CHUNK, dtype=f32)
    rel = idx[:, None] - idx[None, :]
    dmat = jnp.where(rel[None] >= 0, jnp.exp(jnp.maximum(rel, 0.0)[None] * log_gamma[:, None, None]), 0.0)
    q_dec = jnp.exp((idx + 1.0)[:, None] * log_gamma[None, :])[None, :, :, None]
    k_dec = jnp.exp((CHUNK - 1.0 - idx)[:, None] * log_gamma[None, :])[None, :, :, None]
    c_dec = jnp.exp(CHUNK * log_gamma)[None, :, None, None]

    def step(state, inp):
        qc, kc, vc = inp
        scores = jnp.einsum('bthd,bshd->bhts', qc, kc) * dmat[None]
        o = (jnp.einsum('bhts,bshv->bthv', scores, vc)
             + jnp.einsum('bthd,bhdv->bthv', qc, state) * q_dec)
        state = c_dec * state + jnp.einsum('bshd,bshv->bhdv', kc * k_dec, vc)
        return state, o

    state0 = jnp.zeros((b_, C_HEADS, C_HEAD_DIM, C_HEAD_DIM), f32)
    _, o = lax.scan(step, state0, (to_chunks(q), to_chunks(k), to_chunks(v)))
    o = from_chunks(o)
    mu = jnp.mean(o, axis=-1, keepdims=True)
    var = jnp.mean(jnp.square(o - mu), axis=-1, keepdims=True)
    o = (o - mu) * lax.rsqrt(var + EPS)
    return o.reshape(b_, s_, W_C) * norm_g.astype(f32) * jax.nn.silu(zg.astype(f32))


def _real_affine_combine(e1, e2):
    a1, b1 = e1
    a2, b2 = e2
    return a2 * a1, a2 * b1 + b2


def rglru_mixer(z_gate, z_x, conv_w, conv_b, w_a, b_a, w_x, b_x, lam):
    f32 = jnp.float32
    b_, s_, _ = z_x.shape
    xf = z_x.astype(f32)
    cw = conv_w.astype(f32)
    xp = jnp.pad(xf, ((0, 0), (CONV_WIDTH - 1, 0), (0, 0)))
    xc = conv_b.astype(f32)
    for tap in range(CONV_WIDTH):
        xc = xc + xp[:, tap:tap + s_] * cw[tap]
    xb = xc.reshape(b_, s_, D_BLOCKS, D_BLOCK)
    r = jax.nn.sigmoid(jnp.einsum('bshi,hij->bshj', xb, w_a.astype(f32)).reshape(b_, s_, W_D) + b_a.astype(f32))
    i = jax.nn.sigmoid(jnp.einsum('bshi,hij->bshj', xb, w_x.astype(f32)).reshape(b_, s_, W_D) + b_x.astype(f32))
    log_a = -RG_C * r * jax.nn.softplus(-lam.astype(f32))
    a = jnp.exp(log_a)
    u = jnp.sqrt(-jnp.expm1(2.0 * log_a)) * (i * xc)
    _, h = lax.associative_scan(_real_affine_combine, (a, u), axis=1)
    return jax.nn.gelu(z_gate.astype(f32)) * h


def swiglu(h, w1, w3, w2):
    return (jax.nn.silu(h @ w1) * (h @ w3)) @ w2


def moe_swiglu(h, router_w, w1, w3, w2):
    b_, s_, d_ = h.shape
    t = h.reshape(b_ * s_, d_)
    logits = (t @ router_w).astype(jnp.float32)
    top_v, top_i = lax.top_k(logits, TOP_K)
    gates = jax.nn.softmax(top_v, axis=-1)
    combine = jnp.einsum('tk,tke->te', gates, jax.nn.one_hot(top_i, N_EXPERTS, dtype=jnp.float32)).astype(t.dtype)
    out = jnp.zeros_like(t)
    for e in range(N_EXPERTS):
        out = out + combine[:, e:e + 1] * swiglu(t, w1[e], w3[e], w2[e])
    return out.reshape(b_, s_, d_)


def setup_inputs(seed: int = 0) -> dict:
    key = jax.random.key(seed)
    keys = list(jax.random.split(key, 48))
    f32 = jnp.float32

    def nrm(shape, scale):
        return jax.random.normal(keys.pop(), shape, f32) * scale

    def gain(shape):
        return 1.0 + nrm(shape, 0.02)

    n = jnp.arange(S5_STATE, dtype=f32)
    rg_u = jax.random.uniform(keys.pop(), (DEPTH, W_D), f32, 0.9, 0.999)
    rg_s = rg_u ** (1.0 / RG_C)
    return {
        'x': nrm((BATCH, SEQ, D_MODEL), 1.0),
        'norm_mix_g': gain((DEPTH, D_MODEL)),
        'norm_ffn_g': gain((DEPTH, D_MODEL)),
        'final_norm_g': gain((D_MODEL,)),
        'w_in': nrm((DEPTH, D_MODEL, D_IN), D_MODEL ** -0.5),
        'w_out': nrm((DEPTH, D_MIX, D_MODEL), D_MIX ** -0.5),
        'hgrn_lb_logits': nrm((DEPTH, W_A), 0.5),
        'hgrn_norm_g': gain((DEPTH, W_A)),
        's5_lambda_re': -0.5 + nrm((DEPTH, S5_GROUPS, S5_STATE), 0.01),
        's5_lambda_im': math.pi * n + nrm((DEPTH, S5_GROUPS, S5_STATE), 0.01),
        's5_log_dt': jax.random.uniform(keys.pop(), (DEPTH, S5_GROUPS), f32, math.log(1e-3), math.log(1e-1)),
        's5_b_re': nrm((DEPTH, S5_GROUPS, S5_STATE, S5_GROUP), (2.0 * S5_GROUP) ** -0.5),
        's5_b_im': nrm((DEPTH, S5_GROUPS, S5_STATE, S5_GROUP), (2.0 * S5_GROUP) ** -0.5),
        's5_c_re': nrm((DEPTH, S5_GROUPS, S5_GROUP, S5_STATE), (2.0 * S5_STATE) ** -0.5),
        's5_c_im': nrm((DEPTH, S5_GROUPS, S5_GROUP, S5_STATE), (2.0 * S5_STATE) ** -0.5),
        's5_d': nrm((DEPTH, W_B), 0.5),
        's5_glu_w': nrm((DEPTH, W_B, W_B), W_B ** -0.5),
        's5_glu_b': nrm((DEPTH, W_B), 0.01),
        'ret_norm_g': gain((DEPTH, W_C)),
        'rg_conv_w': nrm((DEPTH, CONV_WIDTH, W_D), CONV_WIDTH ** -0.5),
        'rg_conv_b': nrm((DEPTH, W_D), 0.01),
        'rg_w_a': nrm((DEPTH, D_BLOCKS, D_BLOCK, D_BLOCK), D_BLOCK ** -0.5),
        'rg_b_a': nrm((DEPTH, W_D), 0.01),
        'rg_w_x': nrm((DEPTH, D_BLOCKS, D_BLOCK, D_BLOCK), D_BLOCK ** -0.5),
        'rg_b_x': nrm((DEPTH, W_D), 0.01),
        'rg_lambda': jnp.log(rg_s) - jnp.log1p(-rg_s),
        'ffn_w1': nrm((N_DENSE, D_MODEL, D_FF), D_MODEL ** -0.5),
        'ffn_w3': nrm((N_DENSE, D_MODEL, D_FF), D_MODEL ** -0.5),
        'ffn_w2': nrm((N_DENSE, D_FF, D_MODEL), D_FF ** -0.5),
        'router_w': nrm((N_MOE, D_MODEL, N_EXPERTS), D_MODEL ** -0.5),
        'moe_w1': nrm((N_MOE, N_EXPERTS, D_MODEL, D_FF_EXPERT), D_MODEL ** -0.5),
        'moe_w3': nrm((N_MOE, N_EXPERTS, D_MODEL, D_FF_EXPERT), D_MODEL ** -0.5),
        'moe_w2': nrm((N_MOE, N_EXPERTS, D_FF_EXPERT, D_MODEL), D_FF_EXPERT ** -0.5),
    }


def reference(x, norm_mix_g, norm_ffn_g, final_norm_g, w_in, w_out,
              hgrn_lb_logits, hgrn_norm_g,
              s5_lambda_re, s5_lambda_im, s5_log_dt, s5_b_re, s5_b_im, s5_c_re, s5_c_im,
              s5_d, s5_glu_w, s5_glu_b,
              ret_norm_g,
              rg_conv_w, rg_conv_b, rg_w_a, rg_b_a, rg_w_x, rg_b_x, rg_lambda,
              ffn_w1, ffn_w3, ffn_w2,
              router_w, moe_w1, moe_w3, moe_w2):
    lb_p = jax.nn.softmax(hgrn_lb_logits.astype(jnp.float32), axis=0)
    lb_all = jnp.cumsum(lb_p, axis=0) - lb_p[0]
    split_points = [int(p) for p in np.cumsum(IN_SPLITS)[:-1]]
    for layer in range(DEPTH):
        h = rms_norm(x, norm_mix_g[layer])
        z = h @ w_in[layer]
        (a_q, a_f, a_i, a_g, b_u, c_q, c_k, c_v, c_g, d_gate, d_x) = jnp.split(z, split_points, axis=-1)
        o_a = hgrn2_mixer(a_q, a_f, a_i, a_g, lb_all[layer], hgrn_norm_g[layer])
        o_b = s5_mixer(b_u, s5_lambda_re[layer], s5_lambda_im[layer], s5_log_dt[layer],
                       s5_b_re[layer], s5_b_im[layer], s5_c_re[layer], s5_c_im[layer],
                       s5_d[layer], s5_glu_w[layer], s5_glu_b[layer])
        o_c = retention_mixer(c_q, c_k, c_v, c_g, ret_norm_g[layer])
        o_d = rglru_mixer(d_gate, d_x, rg_conv_w[layer], rg_conv_b[layer], rg_w_a[layer], rg_b_a[layer],
                          rg_w_x[layer], rg_b_x[layer], rg_lambda[layer])
        mix = jnp.concatenate([o_a, o_b, o_c, o_d], axis=-1).astype(x.dtype)
        x = x + mix @ w_out[layer]
        h = rms_norm(x, norm_ffn_g[layer])
        if layer % 2 == 0:
            x = x + swiglu(h, ffn_w1[layer // 2], ffn_w3[layer // 2], ffn_w2[layer // 2])
        else:
            x = x + moe_swiglu(h, router_w[layer // 2], moe_w1[layer // 2], moe_w3[layer // 2], moe_w2[layer // 2])
    return rms_norm(x, final_norm_g)
```

```python
import contextlib
import math
import numpy as np
import concourse.bass as bass
import concourse.mybir as mybir
from concourse.bass_utils import run_bass_kernel_spmd

F32 = mybir.dt.float32
AF = mybir.ActivationFunctionType
ALU = mybir.AluOpType
ENGS = ["pe", "act", "dve", "pool", "sp"]
EPS = 1e-6


class Cfg:
    D = 4096
    S = 16384
    DFF = 14336
    DFE = 4096
    NE = 8
    NC = 8
    TBB = 256
    TBC = 256
    FG = 16


class Prog:
    def __init__(self):
        self.nc = bass.Bass("TRN2", target_bir_lowering=False)
        self.ops = {e: [] for e in ENGS}
        self.count = {e: 0 for e in ENGS}
        self.waited = {e: {} for e in ENGS}
        self.lastw = {}
        self.readers = {}
        self.dmacnt = {}
        self.n_ops = 0
        self.rr = 0

    def _collect(self, eng, reads, writes):
        need = {}

        def add(tok):
            s, v = tok
            if eng == "pe" and s == "E_pe":
                return
            if need.get(s, 0) < v:
                need[s] = v

        for k in reads:
            t = self.lastw.get(k)
            if t is not None:
                add(t)
        for k in writes:
            t = self.lastw.get(k)
            if t is not None:
                add(t)
            for r in self.readers.get(k, ()):
                add(r)
        out = []
        w = self.waited[eng]
        for s, v in need.items():
            if w.get(s, 0) < v:
                w[s] = v
                out.append((s, v))
        return out

    def _record(self, tok, reads, writes):
        for k in reads:
            self.readers.setdefault(k, []).append(tok)
        for k in writes:
            self.lastw[k] = tok
            self.readers[k] = []

    def op(self, eng, fn, reads=(), writes=()):
        waits = self._collect(eng, reads, writes)
        self.count[eng] += 1
        tok = ("E_" + eng, self.count[eng])
        self.ops[eng].append((waits, fn, "E_" + eng, 1))
        self._record(tok, reads, writes)
        self.n_ops += 1

    def dma(self, out, in_, reads=(), writes=(), sem="dma", eng="sp"):
        waits = self._collect(eng, reads, writes)
        s = "D_" + sem
        self.dmacnt[s] = self.dmacnt.get(s, 0) + 16
        tok = (s, self.dmacnt[s])
        self.ops[eng].append((waits, lambda e: e.dma_start(out=out, in_=in_), s, 16))
        self._record(tok, reads, writes)
        self.n_ops += 1

    @staticmethod
    def K(*aps):
        return [a.tensor.name for a in aps if hasattr(a, "tensor")]

    def act(self, out, in_, func, bias=None, scale=None):
        kw = {}
        if bias is not None:
            kw["bias"] = bias
        if scale is not None:
            kw["scale"] = scale
        self.op("act", lambda e: e.activation(out, in_, func, **kw), self.K(in_, bias, scale), self.K(out))

    def tt(self, eng, out, a, b, op):
        self.op(eng, lambda e: e.tensor_tensor(out, a, b, op), self.K(a, b), self.K(out))

    def ts(self, eng, out, a, s1, s2, op0, op1=None):
        if op1 is None:
            self.op(eng, lambda e: e.tensor_scalar(out, a, s1, None, op0), self.K(a, s1), self.K(out))
        else:
            self.op(eng, lambda e: e.tensor_scalar(out, a, s1, s2, op0, op1), self.K(a, s1, s2), self.K(out))

    def stt(self, eng, out, in0, scalar, in1, op0, op1):
        eng = "dve"
        self.op(eng, lambda e: e.scalar_tensor_tensor(out, in0, scalar, in1, op0, op1),
                self.K(in0, scalar, in1), self.K(out))

    def mm(self, out, lhsT, rhs, start=True, stop=True):
        self.op("pe", lambda e: e.matmul(out, lhsT, rhs, start=start, stop=stop), self.K(lhsT, rhs), self.K(out))

    def tr(self, out, in_, ident):
        self.op("pe", lambda e: e.transpose(out, in_, ident), self.K(in_, ident), self.K(out))

    def copy(self, eng, out, in_):
        if eng == "act":
            self.op("act", lambda e: e.activation(out, in_, AF.Copy), self.K(in_), self.K(out))
        else:
            self.op(eng, lambda e: e.tensor_copy(out, in_), self.K(in_), self.K(out))

    def scan(self, out, d0, d1, init, op0, op1):
        self.op("dve", lambda e: e.tensor_tensor_scan(out, d0, d1, init, op0, op1), self.K(d0, d1, init), self.K(out))

    def recip(self, out, in_):
        self.op("dve", lambda e: e.reciprocal(out, in_), self.K(in_), self.K(out))

    def memset(self, eng, out, val):
        self.op(eng, lambda e: e.memset(out, val), [], self.K(out))

    def load(self, out, in_):
        self.dma(out, in_, writes=self.K(out), sem="L_" + out.tensor.name)

    def store(self, out, in_):
        self.dma(out, in_, reads=self.K(in_), sem="S_" + in_.tensor.name)

    def alt(self):
        self.rr ^= 1
        return "dve" if self.rr else "pool"

    def emit(self):
        nc = self.nc
        names = ["E_" + e for e in ENGS] + sorted(self.dmacnt)
        with contextlib.ExitStack() as st:
            sems = {n: st.enter_context(nc.semaphore(n)) for n in names}
            block = st.enter_context(nc.Block())

            def run(engname):
                def body(e):
                    for waits, fn, sname, inc in self.ops[engname]:
                        for s, v in waits:
                            e.wait_ge(sems[s], v)
                        fn(e).then_inc(sems[sname], inc)
                    if engname == "sp":
                        for s in sorted(self.dmacnt):
                            e.wait_ge(sems[s], self.dmacnt[s])
                        for en in ENGS:
                            if en != "sp" and self.count[en] > 0:
                                e.wait_ge(sems["E_" + en], self.count[en])
                return body

            block.sync(run("sp"))
            block.tensor(run("pe"))
            block.scalar(run("act"))
            block.vector(run("dve"))
            block.gpsimd(run("pool"))
        return nc


def gelu_tanh(P, st_alloc, x, out, tmp1, tmp2):
    P.tt("pool", tmp1, x, x, ALU.mult)
    P.ts("dve", tmp1, tmp1, 0.044715, 1.0, ALU.mult, ALU.add)
    P.tt("pool", tmp1, tmp1, x, ALU.mult)
    P.act(tmp2, tmp1, AF.Sigmoid, scale=1.5957691216057308)
    P.tt("dve", out, x, tmp2, ALU.mult)


ZQ, ZF, ZI, ZG, ZU, RQ0, RQ1, RK0, RK1, RV, RG_, GG, GX = range(13)


def build_B(cfg):
    D, S, TB = cfg.D, cfg.S, cfg.TBB
    KT = D // 128
    NBLK = S // TB
    NCH = TB // 64
    P = Prog()
    nc = P.nc

    def din(name, shape):
        return nc.dram_tensor(name, shape, F32, kind="ExternalInput").ap()

    xT = din("xT", [D, S])
    gmix = din("gmix", [128, KT])
    win = din("win", [13, 128, KT, 128])
    ones_d = din("ones", [128, 128])
    ident_d = din("ident", [128, 128])
    maskT_d = din("maskT", [64, 64])
    hgp = din("hgp", [128, 4])
    s5s = din("s5s", [128, 12])
    s5bd = din("s5bd", [7, 4, 128, 128])
    s5d = din("s5d", [128, 1])
    cos_d = din("cosT", [128, S])
    sin_d = din("sinT", [128, S])
    retc = din("retc", [128, 2 * TB + 1])
    dmat_d = din("dmatT", [64, 64])
    rgp = din("rgp", [128, 8])
    rgw = din("rgw", [2, 128, 128])
    mix = nc.dram_tensor("mix", [5, 128, S], F32, kind="ExternalOutput").ap()

    with contextlib.ExitStack() as st:
        def sb(name, shape):
            return st.enter_context(nc.sbuf_tensor(name, shape, F32))

        def pb(name):
            return st.enter_context(nc.psum_tensor(name, [128, TB], F32))

        ones = sb("ones_s", [128, 128]); P.load(ones[:, :], ones_d)
        ident = sb("ident_s", [128, 128]); P.load(ident[:, :], ident_d)
        maskT = sb("maskT_s", [64, 64]); P.load(maskT[:, :], maskT_d)
        dmatT = sb("dmatT_s", [64, 64]); P.load(dmatT[:, :], dmat_d)
        gm = sb("gm_s", [128, KT]); P.load(gm[:, :], gmix)
        hg = sb("hg_s", [128, 4]); P.load(hg[:, :], hgp)
        s5st = sb("s5st", [128, 12]); P.load(s5st[:, :], s5s)
        s5dd = sb("s5dd", [128, 1]); P.load(s5dd[:, :], s5d)
        rc = sb("retc_s", [128, 2 * TB + 1]); P.load(rc[:, :], retc)
        rg = sb("rgp_s", [128, 8]); P.load(rg[:, :], rgp)
        wa = sb("wa_s", [128, 128]); P.load(wa[:, :], rgw[0])
        wx = sb("wx_s", [128, 128]); P.load(wx[:, :], rgw[1])
        onesT = sb("onesT", [128, TB]); P.memset("pool", onesT[:, :], 1.0)

        pss = pb("pss"); pz = [pb("pz0"), pb("pz1")]
        pm = [pb(f"pm{i}") for i in range(5)]

        hgc = sb("hgc", [128, 4])
        P.tt("dve", hgc[:, 0:1], hg[:, 1:2], hg[:, 0:1], ALU.subtract)
        P.act(hgc[:, 1:2], hgc[:, 0:1], AF.Sigmoid)
        P.tt("dve", hgc[:, 2:3], hgc[:, 1:2], hg[:, 2:3], ALU.mult)
        P.ts("dve", hgc[:, 3:4], hgc[:, 2:3], -1.0, 1.0, ALU.mult, ALU.add)
        lb_c, oml_c, hng_c = hgc[:, 2:3], hgc[:, 3:4], hg[:, 3:4]

        rgc = sb("rgc", [128, 2])
        P.act(rgc[:, 0:1], rg[:, 7:8], AF.Exp, scale=-1.0)
        P.act(rgc[:, 1:2], rgc[:, 0:1], AF.Ln, bias=1.0)
        P.ts("dve", rgc[:, 1:2], rgc[:, 1:2], -8.0, None, ALU.mult)
        rg_c = rgc[:, 1:2]

        TWO_PI = 2.0 * math.pi

        def s5_disc(tag, lre, lim, ldt, F, want_coef):
            t = {n: sb(f"{tag}_{n}", [128, F]) for n in
                 ["dt", "mag", "ang", "r", "cos", "sin", "abr", "abi", "den", "nr", "t1", "t2", "cr", "ci"]}
            P.act(t["dt"][:, :], ldt, AF.Exp)
            P.tt("dve", t["mag"][:, :], lre, t["dt"][:, :], ALU.mult)
            P.act(t["mag"][:, :], t["mag"][:, :], AF.Exp)
            P.tt("dve", t["ang"][:, :], lim, t["dt"][:, :], ALU.mult)
            ti = st.enter_context(nc.sbuf_tensor(f"{tag}_int", [128, F], mybir.dt.int32))
            for (dst, shift) in (("sin", 0.0), ("cos", 0.5 * math.pi)):
                P.ts("dve", t["t1"][:, :], t["ang"][:, :], shift, None, ALU.add)
                P.ts("dve", t["r"][:, :], t["t1"][:, :], 1.0 / TWO_PI, None, ALU.mult)
                P.copy("dve", ti[:, :], t["r"][:, :])
                P.copy("dve", t["r"][:, :], ti[:, :])
                P.stt("dve", t["r"][:, :], t["r"][:, :], -TWO_PI, t["t1"][:, :], ALU.mult, ALU.add)
                P.act(t[dst][:, :], t["r"][:, :], AF.Sin)
            P.tt("dve", t["abr"][:, :], t["mag"][:, :], t["cos"][:, :], ALU.mult)
            P.tt("dve", t["abi"][:, :], t["mag"][:, :], t["sin"][:, :], ALU.mult)
            if want_coef:
                P.tt("dve", t["den"][:, :], lre, lre, ALU.mult)
                P.tt("dve", t["t1"][:, :], lim, lim, ALU.mult)
                P.tt("dve", t["den"][:, :], t["den"][:, :], t["t1"][:, :], ALU.add)
                P.recip(t["den"][:, :], t["den"][:, :])
                P.ts("dve", t["nr"][:, :], t["abr"][:, :], -1.0, None, ALU.add)
                P.tt("dve", t["t1"][:, :], t["nr"][:, :], lre, ALU.mult)
                P.tt("dve", t["t2"][:, :], t["abi"][:, :], lim, ALU.mult)
                P.tt("dve", t["t1"][:, :], t["t1"][:, :], t["t2"][:, :], ALU.add)
                P.tt("dve", t["cr"][:, :], t["t1"][:, :], t["den"][:, :], ALU.mult)
                P.tt("dve", t["t1"][:, :], t["abi"][:, :], lre, ALU.mult)
                P.tt("dve", t["t2"][:, :], t["nr"][:, :], lim, ALU.mult)
                P.tt("dve", t["t1"][:, :], t["t1"][:, :], t["t2"][:, :], ALU.subtract)
                P.tt("dve", t["ci"][:, :], t["t1"][:, :], t["den"][:, :], ALU.mult)
            return t

        ds_ = s5_disc("s5a", s5st[:, 0:4], s5st[:, 4:8], s5st[:, 8:12], 4, False)
        NSTEP = int(math.log2(TB))
        pw = [sb(f"s5pw{k}", [128, 12]) for k in range(NSTEP)]
        P.copy("dve", pw[0][:, 0:4], ds_["abr"][:, :])
        P.copy("dve", pw[0][:, 4:8], ds_["abi"][:, :])
        P.ts("dve", pw[0][:, 8:12], ds_["abi"][:, :], -1.0, None, ALU.mult)
        s5tmp = sb("s5tmp", [128, 8])
        for k in range(1, NSTEP):
            a, b = pw[k - 1][:, 0:4], pw[k - 1][:, 4:8]
            P.tt("dve", s5tmp[:, 0:4], a, a, ALU.mult)
            P.tt("dve", s5tmp[:, 4:8], b, b, ALU.mult)
            P.tt("dve", pw[k][:, 0:4], s5tmp[:, 0:4], s5tmp[:, 4:8], ALU.subtract)
            P.tt("dve", s5tmp[:, 0:4], a, b, ALU.mult)
            P.ts("dve", pw[k][:, 4:8], s5tmp[:, 0:4], 2.0, None, ALU.mult)
            P.ts("dve", pw[k][:, 8:12], s5tmp[:, 0:4], -2.0, None, ALU.mult)
        BDre, BDim, CDre, CDimn = [], [], [], []
        for j in range(4):
            ld = [sb(f"s5ld{j}_{i}", [128, 128]) for i in range(7)]
            for i in range(7):
                P.load(ld[i][:, :], s5bd[i, j])
            dd = s5_disc(f"s5b{j}", ld[0][:, :], ld[1][:, :], ld[2][:, :], 128, True)
            bre = sb(f"BDre{j}", [128, 128]); bim = sb(f"BDim{j}", [128, 128])
            t1 = dd["t1"][:, :]; t2 = dd["t2"][:, :]
            P.tt("dve", t1, dd["cr"][:, :], ld[3][:, :], ALU.mult)
            P.tt("dve", t2, dd["ci"][:, :], ld[4][:, :], ALU.mult)
            P.tt("dve", bre[:, :], t1, t2, ALU.subtract)
            P.tt("dve", t1, dd["cr"][:, :], ld[4][:, :], ALU.mult)
            P.tt("dve", t2, dd["ci"][:, :], ld[3][:, :], ALU.mult)
            P.tt("dve", bim[:, :], t1, t2, ALU.add)
            P.ts("dve", ld[6][:, :], ld[6][:, :], -1.0, None, ALU.mult)
            BDre.append(bre); BDim.append(bim); CDre.append(ld[5]); CDimn.append(ld[6])

        xpad = sb("xpad", [128, TB + 3]); P.memset("pool", xpad[:, :], 0.0)
        hlast = sb("hlast", [128, 1]); P.memset("pool", hlast[:, :], 0.0)
        s5car = sb("s5car", [128, 8]); P.memset("pool", s5car[:, :], 0.0)
        Sh = sb("Sh", [128, 128]); P.memset("pool", Sh[:, :], 0.0)
        Sa = sb("Sa", [128, 128]); P.memset("pool", Sa[:, :], 0.0)
        Sb = sb("Sb", [128, 128]); P.memset("pool", Sb[:, :], 0.0)

        xs = sb("xs", [128, KT, TB])
        sq = [sb("sq0", [128, TB]), sb("sq1", [128, TB])]
        rstd = sb("rstd", [128, TB])
        wt = [sb("wt0", [128, KT, 128]), sb("wt1", [128, KT, 128])]
        zs = [sb(f"z{i}", [128, TB]) for i in range(13)]
        W = [sb(f"w{i}", [128, TB]) for i in range(13)]
        s5b = [[sb(f"s5x{j}_{i}", [128, TB]) for i in range(4)] for j in range(4)]
        small = [sb(f"sm{i}", [64, 128]) for i in range(6)]
        scT = sb("scT", [64, 64])
        cosb = sb("cosb", [128, TB]); sinb = sb("sinb", [128, TB])
        nmid = sb("nmid", [128, NCH])
        ob = [sb(f"ob{i}", [128, TB]) for i in range(5)]

        for blk in range(NBLK):
            ts_ = slice(blk * TB, (blk + 1) * TB)
            P.load(xs[:, :, :], xT[:, ts_].rearrange("(kt p) t -> p kt t", p=128))
            for kt in range(KT):
                s = sq[kt % 2]
                P.act(s[:, :], xs[:, kt, :], AF.Square)
                P.mm(pss[:, :], ones[:, :], s[:, :], start=(kt == 0), stop=(kt == KT - 1))
            P.act(rstd[:, :], pss[:, :], AF.Sqrt, bias=EPS, scale=1.0 / D)
            P.recip(rstd[:, :], rstd[:, :])
            for kt in range(KT):
                P.stt(P.alt(), xs[:, kt, :], xs[:, kt, :], gm[:, kt:kt + 1], rstd[:, :], ALU.mult, ALU.mult)
            for ct in range(13):
                b = ct % 2
                P.load(wt[b][:, :, :], win[ct])
                for kt in range(KT):
                    P.mm(pz[b][:, :], wt[b][:, kt, :], xs[:, kt, :], start=(kt == 0), stop=(kt == KT - 1))
                P.copy("act", zs[ct][:, :], pz[b][:, :])

            G, X = zs[GG], zs[GX]
            xc, r_, i_, a_, t1, t2 = W[0], W[1], W[2], W[3], W[4], W[5]
            P.copy("pool", xpad[:, 3:TB + 3], X[:, :])
            P.ts("dve", xc[:, :], xpad[:, 0:TB], rg[:, 0:1], rg[:, 4:5], ALU.mult, ALU.add)
            for tap in range(1, 4):
                P.stt("dve", xc[:, :], xpad[:, tap:tap + TB], rg[:, tap:tap + 1], xc[:, :], ALU.mult, ALU.add)
            P.copy("pool", xpad[:, 0:3], xpad[:, TB:TB + 3])
            P.mm(pm[0][:, :], wa[:, :], xc[:, :])
            P.act(r_[:, :], pm[0][:, :], AF.Sigmoid, bias=rg[:, 5:6])
            P.mm(pm[1][:, :], wx[:, :], xc[:, :])
            P.act(i_[:, :], pm[1][:, :], AF.Sigmoid, bias=rg[:, 6:7])
            P.act(a_[:, :], r_[:, :], AF.Exp, scale=rg_c)
            P.tt("pool", t1[:, :], a_[:, :], a_[:, :], ALU.mult)
            P.act(t1[:, :], t1[:, :], AF.Sqrt, bias=1.0, scale=-1.0)
            P.tt("pool", t2[:, :], i_[:, :], xc[:, :], ALU.mult)
            P.tt("pool", t2[:, :], t2[:, :], t1[:, :], ALU.mult)
            P.scan(r_[:, :], a_[:, :], t2[:, :], hlast[:, 0:1], ALU.mult, ALU.add)
            P.copy("act", hlast[:, 0:1], r_[:, TB - 1:TB])
            gelu_tanh(P, None, G[:, :], i_[:, :], t1[:, :], t2[:, :])
            P.tt("dve", ob[4][:, :], i_[:, :], r_[:, :], ALU.mult)
            P.store(mix[4, :, ts_], ob[4][:, :])

            U = zs[ZU]
            fin = []
            for j in range(4):
                eng = "dve" if j % 2 == 0 else "pool"
                Ar, Ai, Br, Bi = s5b[j]
                P.mm(pm[0 + (j % 2) * 2][:, :], BDre[j][:, :], U[:, :])
                P.copy("act", Ar[:, :], pm[0 + (j % 2) * 2][:, :])
                P.mm(pm[1 + (j % 2) * 2][:, :], BDim[j][:, :], U[:, :])
                P.copy("act", Ai[:, :], pm[1 + (j % 2) * 2][:, :])
                abr, abi, nabi = pw[0][:, j:j + 1], pw[0][:, 4 + j:5 + j], pw[0][:, 8 + j:9 + j]
                cr, ci = s5car[:, j:j + 1], s5car[:, 4 + j:5 + j]
                P.stt(eng, Ar[:, 0:1], cr, abr, Ar[:, 0:1], ALU.mult, ALU.add)
                P.stt(eng, Ar[:, 0:1], ci, nabi, Ar[:, 0:1], ALU.mult, ALU.add)
                P.stt(eng, Ai[:, 0:1], ci, abr, Ai[:, 0:1], ALU.mult, ALU.add)
                P.stt(eng, Ai[:, 0:1], cr, abi, Ai[:, 0:1], ALU.mult, ALU.add)
                cur, nxt = (Ar, Ai), (Br, Bi)
                for k in range(NSTEP):
                    d = 1 << k
                    pr, pi, npi = pw[k][:, j:j + 1], pw[k][:, 4 + j:5 + j], pw[k][:, 8 + j:9 + j]
                    cr_, ci_ = cur
                    nr_, ni_ = nxt
                    P.stt(eng, nr_[:, d:TB], cr_[:, 0:TB - d], pr, cr_[:, d:TB], ALU.mult, ALU.add)
                    P.stt(eng, nr_[:, d:TB], ci_[:, 0:TB - d], npi, nr_[:, d:TB], ALU.mult, ALU.add)
                    P.stt(eng, ni_[:, d:TB], ci_[:, 0:TB - d], pr, ci_[:, d:TB], ALU.mult, ALU.add)
                    P.stt(eng, ni_[:, d:TB], cr_[:, 0:TB - d], pi, ni_[:, d:TB], ALU.mult, ALU.add)
                    P.copy("act", nr_[:, 0:d], cr_[:, 0:d])
                    P.copy("act", ni_[:, 0:d], ci_[:, 0:d])
                    cur, nxt = nxt, cur
                fin.append(cur)
                P.copy("act", s5car[:, j:j + 1], cur[0][:, TB - 1:TB])
                P.copy("act", s5car[:, 4 + j:5 + j], cur[1][:, TB - 1:TB])
            for j in range(4):
                P.mm(pm[4][:, :], CDre[j][:, :], fin[j][0][:, :], start=(j == 0), stop=False)
                P.mm(pm[4][:, :], CDimn[j][:, :], fin[j][1][:, :], start=False, stop=(j == 3))
            y2, t1, t2 = W[0], W[1], W[2]
            P.stt("dve", y2[:, :], U[:, :], s5dd[:, 0:1], pm[4][:, :], ALU.mult, ALU.add)
            gelu_tanh(P, None, y2[:, :], ob[1][:, :], t1[:, :], t2[:, :])
            P.store(mix[1, :, ts_], ob[1][:, :])

            Q, Fz, I, G = zs[ZQ], zs[ZF], zs[ZI], zs[ZG]
            qs, f_, lf, k_, cum, e1, e2, e3, ecum = W[3], W[4], W[5], W[6], W[7], W[8], W[9], W[10], W[11]
            P.act(qs[:, :], Q[:, :], AF.Silu)
            P.act(f_[:, :], Fz[:, :], AF.Sigmoid)
            P.ts("dve", f_[:, :], f_[:, :], oml_c, lb_c, ALU.mult, ALU.add)
            P.act(lf[:, :], f_[:, :], AF.Ln)
            P.ts("pool", k_[:, :], f_[:, :], -1.0, 1.0, ALU.mult, ALU.add)
            for c in range(NCH):
                cs = slice(c * 64, (c + 1) * 64)
                P.scan(cum[:, cs], onesT[:, 0:64], lf[:, cs], 0.0, ALU.mult, ALU.add)
            cum3 = cum[:, :].rearrange("p (c l) -> p c l", l=64)
            P.ts("dve", nmid[:, :], cum3[:, :, 31], -1.0, None, ALU.mult)
            for c in range(NCH):
                cs = slice(c * 64, (c + 1) * 64)
                mid = cum[:, c * 64 + 31:c * 64 + 32]
                last = cum[:, c * 64 + 63:c * 64 + 64]
                P.act(e1[:, cs], cum[:, cs], AF.Exp, bias=nmid[:, c:c + 1], scale=1.0)
                P.act(e2[:, cs], cum[:, cs], AF.Exp, bias=mid, scale=-1.0)
                P.act(e3[:, cs], cum[:, cs], AF.Exp, bias=last, scale=-1.0)
            P.act(ecum[:, :], cum[:, :], AF.Exp)
            P.tt("dve", e1[:, :], e1[:, :], qs[:, :], ALU.mult)
            P.tt("pool", e2[:, :], e2[:, :], k_[:, :], ALU.mult)
            P.tt("pool", e3[:, :], e3[:, :], k_[:, :], ALU.mult)
            P.tt("dve", qs[:, :], qs[:, :], ecum[:, :], ALU.mult)
            for c in range(NCH):
                cs = slice(c * 64, (c + 1) * 64)
                Kun, Vn = small[0], small[1]
                P.tr(pm[0][0:64, 0:128], e3[:, cs], ident[:, :])
                P.copy("act", Kun[:, :], pm[0][0:64, 0:128])
                P.tr(pm[1][0:64, 0:128], I[:, cs], ident[:, :])
                P.copy("act", Vn[:, :], pm[1][0:64, 0:128])
                P.mm(pm[2][0:64, 0:64], e2[:, cs], e1[:, cs])
                P.tt("dve", scT[:, :], pm[2][0:64, 0:64], maskT[:, :], ALU.mult)
                P.mm(pm[4][:, cs], Vn[:, :], scT[:, :], start=True, stop=False)
                P.mm(pm[4][:, cs], Sh[:, :], qs[:, cs], start=False, stop=True)
                P.mm(pm[3][:, 0:128], Kun[:, :], Vn[:, :])
                P.stt("dve", Sh[:, :], Sh[:, :], ecum[:, c * 64 + 63:c * 64 + 64], pm[3][:, 0:128], ALU.mult, ALU.add)
            osb, t1, t2 = W[0], W[1], W[2]
            P.copy("act", osb[:, :], pm[4][:, :])
            P.tt("pool", t1[:, :], osb[:, :], osb[:, :], ALU.mult)
            P.mm(pm[0][:, :], ones[:, :], t1[:, :])
            P.act(t2[:, :], pm[0][:, :], AF.Sqrt, bias=EPS, scale=1.0 / 128.0)
            P.recip(t2[:, :], t2[:, :])
            P.stt("dve", osb[:, :], osb[:, :], hng_c, t2[:, :], ALU.mult, ALU.mult)
            P.act(t1[:, :], G[:, :], AF.Silu)
            P.tt("dve", ob[0][:, :], osb[:, :], t1[:, :], ALU.mult)
            P.store(mix[0, :, ts_], ob[0][:, :])

            P.load(cosb[:, :], cos_d[:, ts_])
            P.load(sinb[:, :], sin_d[:, ts_])
            qa, qb, ka, kb, t1, t2 = W[3], W[4], W[5], W[6], W[7], W[8]
            for (x0, x1, oa, ob_) in ((zs[RQ0], zs[RQ1], qa, qb), (zs[RK0], zs[RK1], ka, kb)):
                P.tt("dve", t1[:, :], x0[:, :], cosb[:, :], ALU.mult)
                P.tt("pool", t2[:, :], x1[:, :], sinb[:, :], ALU.mult)
                P.tt("dve", oa[:, :], t1[:, :], t2[:, :], ALU.subtract)
                P.tt("pool", t1[:, :], x0[:, :], sinb[:, :], ALU.mult)
                P.tt("dve", t2[:, :], x1[:, :], cosb[:, :], ALU.mult)
                P.tt("pool", ob_[:, :], t1[:, :], t2[:, :], ALU.add)
            qia, qib, kua, kub = W[9], W[10], W[11], W[12]
            P.tt("dve", qia[:, :], qa[:, :], rc[:, 0:TB], ALU.mult)
            P.tt("pool", qib[:, :], qb[:, :], rc[:, 0:TB], ALU.mult)
            P.tt("dve", kua[:, :], ka[:, :], rc[:, TB:2 * TB], ALU.mult)
            P.tt("pool", kub[:, :], kb[:, :], rc[:, TB:2 * TB], ALU.mult)
            cdec = rc[:, 2 * TB:2 * TB + 1]
            Vz = zs[RV]
            for c in range(NCH):
                cs = slice(c * 64, (c + 1) * 64)
                Kna, Knb, Vn = small[2], small[3], small[4]
                P.tr(pm[0][0:64, 0:128], kua[:, cs], ident[:, :])
                P.copy("act", Kna[:, :], pm[0][0:64, 0:128])
                P.tr(pm[1][0:64, 0:128], kub[:, cs], ident[:, :])
                P.copy("act", Knb[:, :], pm[1][0:64, 0:128])
                P.tr(pm[2][0:64, 0:128], Vz[:, cs], ident[:, :])
                P.copy("act", Vn[:, :], pm[2][0:64, 0:128])
                P.mm(pm[3][0:64, 0:64], ka[:, cs], qa[:, cs], start=True, stop=False)
                P.mm(pm[3][0:64, 0:64], kb[:, cs], qb[:, cs], start=False, stop=True)
                P.tt("dve", scT[:, :], pm[3][0:64, 0:64], dmatT[:, :], ALU.mult)
                P.mm(pm[4][:, cs], Vn[:, :], scT[:, :], start=True, stop=False)
                P.mm(pm[4][:, cs], Sa[:, :], qia[:, cs], start=False, stop=False)
                P.mm(pm[4][:, cs], Sb[:, :], qib[:, cs], start=False, stop=True)
                P.mm(pm[0][:, 0:128], Kna[:, :], Vn[:, :])
                P.stt("dve", Sa[:, :], Sa[:, :], cdec, pm[0][:, 0:128], ALU.mult, ALU.add)
                P.mm(pm[1][:, 0:128], Knb[:, :], Vn[:, :])
                P.stt("dve", Sb[:, :], Sb[:, :], cdec, pm[1][:, 0:128], ALU.mult, ALU.add)
            P.copy("act", ob[2][:, :], pm[4][:, :])
            P.store(mix[2, :, ts_], ob[2][:, :])
            P.act(ob[3][:, :], zs[RG_][:, :], AF.Silu)
            P.store(mix[3, :, ts_], ob[3][:, :])
        P.emit()
    return P


def _tiles_lhsT(w, KT):
    K, M = w.shape
    return np.ascontiguousarray(w.reshape(KT, 128, M // 128, 128).transpose(2, 1, 0, 3))


def _ret_consts(h, TB):
    f32 = np.float32
    lg = np.log(f32(1.0) - f32(2.0) ** (f32(-5.0) - f32(h))).astype(f32)
    idx = np.arange(64, dtype=f32)
    rel = idx[None, :] - idx[:, None]
    dmatT = np.where(rel >= 0, np.exp(np.maximum(rel, 0) * lg), 0.0).astype(f32) * f32(256 ** -0.5)
    qdec = np.exp((idx + 1.0) * lg).astype(f32)
    kdec = (np.exp((63.0 - idx) * lg) * f32(256 ** -0.5)).astype(f32)
    cdec = np.exp(f32(64.0) * lg).astype(f32)
    rc = np.zeros((128, 2 * TB + 1), f32)
    rc[:, 0:TB] = np.tile(qdec, TB // 64)[None, :]
    rc[:, TB:2 * TB] = np.tile(kdec, TB // 64)[None, :]
    rc[:, 2 * TB] = cdec
    return dmatT.astype(f32), rc


def prep_B(inp, layer, cfg, xT):
    D, S = cfg.D, cfg.S
    KT = D // 128
    f32 = np.float32
    w_in = inp["w_in"][layer]
    ones = np.ones((128, 128), f32)
    ident = np.eye(128, dtype=f32)
    maskT = np.triu(np.ones((64, 64), f32))
    pos = np.arange(S, dtype=f32)
    inv_freq = (f32(10000.0) ** (-np.arange(0, 256, 2, dtype=f32) / f32(256))).astype(f32)
    ang = (pos[None, :] * inv_freq[:, None]).astype(f32)
    cosT = np.cos(ang).astype(f32)
    sinT = np.sin(ang).astype(f32)
    gmix = np.ascontiguousarray(inp["norm_mix_g"][layer].reshape(KT, 128).T)
    maps = []
    for c in range(8):
        h, half = c // 2, c % 2
        cols = [0 + c * 128, 1024 + c * 128, 2048 + c * 128, 3072 + c * 128, 4096 + c * 128,
                5120 + h * 256, 5120 + h * 256 + 128, 6144 + h * 256, 6144 + h * 256 + 128,
                7168 + h * 256 + half * 128, 8192 + h * 256 + half * 128,
                9216 + c * 128, 10240 + c * 128]
        wsel = np.concatenate([w_in[:, c0:c0 + 128] for c0 in cols], axis=1)
        win = _tiles_lhsT(wsel, KT)
        ch = slice(c * 128, (c + 1) * 128)
        hgp = np.stack([inp["hgrn_lb_logits"][0, ch], inp["hgrn_lb_logits"][1, ch],
                        np.full(128, float(layer), f32), inp["hgrn_norm_g"][layer, ch]], axis=1).astype(f32)
        s5s = np.zeros((128, 12), f32)
        s5bd = np.zeros((7, 4, 128, 128), f32)
        for j in range(4):
            for gl in range(2):
                gg = 2 * j + gl
                g = 8 * c + gg
                ps = slice(gl * 64, gl * 64 + 64)
                s5s[ps, j] = inp["s5_lambda_re"][layer, g]
                s5s[ps, 4 + j] = inp["s5_lambda_im"][layer, g]
                s5s[ps, 8 + j] = inp["s5_log_dt"][layer, g]
                s5bd[0, j, :, ps] = inp["s5_lambda_re"][layer, g][None, :]
                s5bd[1, j, :, ps] = inp["s5_lambda_im"][layer, g][None, :]
                s5bd[2, j, :, ps] = inp["s5_log_dt"][layer, g]
                rs = slice(gg * 16, gg * 16 + 16)
                s5bd[3, j, rs, ps] = inp["s5_b_re"][layer, g].T
                s5bd[4, j, rs, ps] = inp["s5_b_im"][layer, g].T
                s5bd[5, j, ps, rs] = inp["s5_c_re"][layer, g].T
                s5bd[6, j, ps, rs] = inp["s5_c_im"][layer, g].T
        s5d = np.ascontiguousarray(inp["s5_d"][layer, ch].reshape(128, 1))
        dmatT, rc = _ret_consts(h, cfg.TBB)
        rgp = np.stack([inp["rg_conv_w"][layer, 0, ch], inp["rg_conv_w"][layer, 1, ch],
                        inp["rg_conv_w"][layer, 2, ch], inp["rg_conv_w"][layer, 3, ch],
                        inp["rg_conv_b"][layer, ch], inp["rg_b_a"][layer, ch],
                        inp["rg_b_x"][layer, ch], inp["rg_lambda"][layer, ch]], axis=1).astype(f32)
        rgw = np.stack([inp["rg_w_a"][layer, c], inp["rg_w_x"][layer, c]]).astype(f32)
        maps.append(dict(xT=xT, gmix=gmix, win=win, ones=ones, ident=ident, maskT=maskT, hgp=hgp,
                         s5s=s5s, s5bd=s5bd, s5d=s5d, cosT=cosT, sinT=sinT, retc=rc, dmatT=dmatT,
                         rgp=rgp, rgw=rgw))
    return maps


def build_C(cfg, moe, last):
    D, TB, FG = cfg.D, cfg.TBC, cfg.FG
    TC = cfg.S // cfg.NC
    KT = D // 128
    DT = KT
    NBLK = TC // TB
    NF = (cfg.NE * cfg.DFE if moe else cfg.DFF) // 128
    NG = NF // FG
    FPE = cfg.DFE // 128
    P = Prog()
    nc = P.nc

    def din(name, shape):
        return nc.dram_tensor(name, shape, F32, kind="ExternalInput").ap()

    xT = din("xT", [D, TC])
    mixin = din("mixin", [40, 128, TC])
    gluw = din("gluw", [8, 128, 8, 128])
    cvec = din("cvec", [128, 16 + 2 * KT])
    wout = din("wout", [DT, 128, 32, 128])
    w1 = din("w1", [NF, 128, KT, 128])
    w3 = din("w3", [NF, 128, KT, 128])
    w2 = din("w2", [NG, DT, 128, FG, 128])
    ones_d = din("ones", [128, 128])
    ident_d = din("ident", [128, 128])
    if moe:
        rw = din("rw", [128, KT, 8])
    outT = nc.dram_tensor("outT", [D, TC], F32, kind="ExternalOutput").ap()

    with contextlib.ExitStack() as st:
        def sb(name, shape):
            return st.enter_context(nc.sbuf_tensor(name, shape, F32))

        def pb(name):
            return st.enter_context(nc.psum_tensor(name, [128, TB], F32))

        ones = sb("ones_s", [128, 128]); P.load(ones[:, :], ones_d)
        ident = sb("ident_s", [128, 128]); P.load(ident[:, :], ident_d)
        cv = sb("cv_s", [128, 16 + 2 * KT]); P.load(cv[:, :], cvec)
        if moe:
            rws = sb("rw_s", [128, KT, 8]); P.load(rws[:, :, :], rw)
        pss = pb("pss"); pz = [pb("pz0"), pb("pz1")]
        pa = [pb("pa0"), pb("pa1")]; pbb = [pb("pb0"), pb("pb1")]; pr = pb("pr")

        xs = sb("xs", [128, KT, TB])
        hs = sb("hs", [128, max(KT, 24), TB])
        mixs = sb("mixs", [128, 32, TB])
        wtA = [sb("wtA0", [128, max(KT, 32), 128]), sb("wtA1", [128, max(KT, 32), 128])]
        wtB = [sb("wtB0", [128, KT, 128]), sb("wtB1", [128, KT, 128])]
        w2b = [sb("w2b0", [128, FG, 128]), sb("w2b1", [128, FG, 128])]
        sq = [sb("sq0", [128, TB]), sb("sq1", [128, TB])]
        rstd = sb("rstd", [128, TB])
        T = [sb(f"t{i}", [128, TB]) for i in range(6)]
        sil = [sb("sil0", [128, TB]), sb("sil1", [128, TB])]
        if moe:
            cb = [sb(f"cb{e}", [128, TB]) for e in range(8)]
            lg = sb("lg", [128, 8]); l2 = sb("l2", [128, 8]); eq1 = sb("eq1", [128, 8]); eq2 = sb("eq2", [128, 8])
            comb = sb("comb", [128, 8]); mm_ = sb("mm_", [128, 4]); rep = sb("rep", [128, 128])

        def rmsnorm(src, dst, gcol0):
            for kt in range(KT):
                s = sq[kt % 2]
                P.act(s[:, :], src[:, kt, :], AF.Square)
                P.mm(pss[:, :], ones[:, :], s[:, :], start=(kt == 0), stop=(kt == KT - 1))
            P.act(rstd[:, :], pss[:, :], AF.Sqrt, bias=EPS, scale=1.0 / D)
            P.recip(rstd[:, :], rstd[:, :])
            for kt in range(KT):
                P.stt("dve", dst[:, kt, :], src[:, kt, :], cv[:, gcol0 + kt:gcol0 + kt + 1], rstd[:, :], ALU.mult, ALU.mult)

        wi = 0
        for blk in range(NBLK):
            ts_ = slice(blk * TB, (blk + 1) * TB)
            P.load(xs[:, :, :], xT[:, ts_].rearrange("(kt p) t -> p kt t", p=128))
            P.load(mixs[:, 0:8, :], mixin[0:8, :, ts_].rearrange("k p t -> p k t"))
            P.load(mixs[:, 24:32, :], mixin[32:40, :, ts_].rearrange("k p t -> p k t"))
            ys, ro, rgt = hs[:, 0:8, :], hs[:, 8:16, :], hs[:, 16:24, :]
            P.load(hs[:, 0:24, :], mixin[8:32, :, ts_].rearrange("k p t -> p k t"))
            for jt in range(8):
                b = wi % 2; wi += 1
                P.load(wtA[b][:, 0:8, :], gluw[jt])
                for it in range(8):
                    P.mm(pz[b][:, :], wtA[b][:, it, :], hs[:, it, :], start=(it == 0), stop=(it == 7))
                P.act(T[0][:, :], pz[b][:, :], AF.Sigmoid, bias=cv[:, jt:jt + 1])
                P.tt("dve", mixs[:, 8 + jt, :], hs[:, jt, :], T[0][:, :], ALU.mult)
            for h in range(4):
                t0, t1 = 8 + 2 * h, 8 + 2 * h + 1
                P.act(sq[0][:, :], hs[:, t0, :], AF.Square)
                P.act(sq[1][:, :], hs[:, t1, :], AF.Square)
                P.mm(pa[0][:, :], ones[:, :], hs[:, t0, :], start=True, stop=False)
                P.mm(pa[0][:, :], ones[:, :], hs[:, t1, :], start=False, stop=True)
                P.mm(pbb[0][:, :], ones[:, :], sq[0][:, :], start=True, stop=False)
                P.mm(pbb[0][:, :], ones[:, :], sq[1][:, :], start=False, stop=True)
                mean, msq, var = T[1], T[2], T[3]
                P.act(mean[:, :], pa[0][:, :], AF.Copy, scale=1.0 / 256.0)
                P.tt("pool", msq[:, :], mean[:, :], mean[:, :], ALU.mult)
                P.stt("dve", var[:, :], pbb[0][:, :], 1.0 / 256.0, msq[:, :], ALU.mult, ALU.subtract)
                P.act(var[:, :], var[:, :], AF.Sqrt, bias=EPS, scale=1.0)
                P.recip(var[:, :], var[:, :])
                for tix in (t0, t1):
                    P.tt("dve", T[4][:, :], hs[:, tix, :], mean[:, :], ALU.subtract)
                    P.stt("dve", T[4][:, :], T[4][:, :], cv[:, tix:tix + 1], var[:, :], ALU.mult, ALU.mult)
                    P.tt("dve", mixs[:, 8 + tix, :], T[4][:, :], hs[:, 8 + tix, :], ALU.mult)
            for dt in range(DT):
                b = wi % 2; wi += 1
                P.load(wtA[b][:, 0:32, :], wout[dt])
                for kt in range(32):
                    P.mm(pz[b][:, :], wtA[b][:, kt, :], mixs[:, kt, :], start=(kt == 0), stop=(kt == 31))
                P.tt("dve", xs[:, dt, :], xs[:, dt, :], pz[b][:, :], ALU.add)
            rmsnorm(xs, hs, 16)
            if moe:
                for sub in range(TB // 128):
                    ss = slice(sub * 128, (sub + 1) * 128)
                    for kt in range(KT):
                        P.mm(pr[:, 0:8], hs[:, kt, ss], rws[:, kt, :], start=(kt == 0), stop=(kt == KT - 1))
                    P.copy("act", lg[:, :], pr[:, 0:8])
                    P.op("dve", lambda e: e.reduce_max(mm_[:, 0:1], lg[:, :], mybir.AxisListType.X), ["lg"], ["mm_"])
                    P.ts("dve", eq1[:, :], lg[:, :], mm_[:, 0:1], None, ALU.is_equal)
                    P.stt("dve", l2[:, :], eq1[:, :], -1e30, lg[:, :], ALU.mult, ALU.add)
                    P.op("dve", lambda e: e.reduce_max(mm_[:, 1:2], l2[:, :], mybir.AxisListType.X), ["l2"], ["mm_"])
                    P.ts("dve", eq2[:, :], l2[:, :], mm_[:, 1:2], None, ALU.is_equal)
                    P.tt("dve", mm_[:, 2:3], mm_[:, 1:2], mm_[:, 0:1], ALU.subtract)
                    P.act(mm_[:, 3:4], mm_[:, 2:3], AF.Sigmoid)
                    P.act(mm_[:, 2:3], mm_[:, 2:3], AF.Sigmoid, scale=-1.0)
                    P.ts("dve", comb[:, :], eq1[:, :], mm_[:, 2:3], None, ALU.mult)
                    P.stt("dve", comb[:, :], eq2[:, :], mm_[:, 3:4], comb[:, :], ALU.mult, ALU.add)
                    for e_ in range(8):
                        P.ts("dve", rep[:, :], ones[:, :], comb[:, e_:e_ + 1], None, ALU.mult)
                        P.mm(pr[:, 0:128], rep[:, :], ident[:, :])
                        P.copy("act", cb[e_][:, ss], pr[:, 0:128])
            ag = mixs
            for g in range(NG):
                ex = (g * FG) // FPE if moe else 0
                for fi in range(FG):
                    ft = g * FG + fi
                    b = wi % 2; wi += 1
                    P.load(wtA[b][:, 0:KT, :], w1[ft])
                    P.load(wtB[b][:, :, :], w3[ft])
                    for kt in range(KT):
                        P.mm(pa[b][:, :], wtA[b][:, kt, :], hs[:, kt, :], start=(kt == 0), stop=(kt == KT - 1))
                    for kt in range(KT):
                        P.mm(pbb[b][:, :], wtB[b][:, kt, :], hs[:, kt, :], start=(kt == 0), stop=(kt == KT - 1))
                    P.act(sil[b][:, :], pa[b][:, :], AF.Silu)
                    P.tt("dve", ag[:, fi, :], sil[b][:, :], pbb[b][:, :], ALU.mult)
                    if moe:
                        P.tt("pool", ag[:, fi, :], ag[:, fi, :], cb[ex][:, :], ALU.mult)
                for dt in range(DT):
                    b = wi % 2; wi += 1
                    P.load(w2b[b][:, :, :], w2[g, dt])
                    for fi in range(FG):
                        P.mm(pz[b][:, :], w2b[b][:, fi, :], ag[:, fi, :], start=(fi == 0), stop=(fi == FG - 1))
                    P.tt("dve", xs[:, dt, :], xs[:, dt, :], pz[b][:, :], ALU.add)
            if last:
                rmsnorm(xs, hs, 16 + KT)
                P.store(outT[:, ts_].rearrange("(kt p) t -> p kt t", p=128), hs[:, 0:KT, :])
            else:
                P.store(outT[:, ts_].rearrange("(kt p) t -> p kt t", p=128), xs[:, :, :])
        P.emit()
    return P


def prep_C(inp, layer, cfg, xT, mixes, moe):
    f32 = np.float32
    D, FG = cfg.D, cfg.FG
    KT = D // 128
    TC = cfg.S // cfg.NC
    li = layer // 2
    mixall = np.stack(mixes)
    mixall = np.ascontiguousarray(mixall.transpose(1, 0, 2, 3)).reshape(40, 128, cfg.S)
    gluw = _tiles_lhsT(inp["s5_glu_w"][layer], 8)
    cvec = np.zeros((128, 16 + 2 * KT), f32)
    cvec[:, 0:8] = inp["s5_glu_b"][layer].reshape(8, 128).T
    cvec[:, 8:16] = inp["ret_norm_g"][layer].reshape(8, 128).T
    cvec[:, 16:16 + KT] = inp["norm_ffn_g"][layer].reshape(KT, 128).T
    cvec[:, 16 + KT:16 + 2 * KT] = inp["final_norm_g"].reshape(KT, 128).T
    wout = _tiles_lhsT(inp["w_out"][layer], 32)
    if moe:
        w1 = np.concatenate([_tiles_lhsT(inp["moe_w1"][li, e], KT) for e in range(cfg.NE)])
        w3 = np.concatenate([_tiles_lhsT(inp["moe_w3"][li, e], KT) for e in range(cfg.NE)])
        w2full = inp["moe_w2"][li].reshape(cfg.NE * cfg.DFE, D)
    else:
        w1 = _tiles_lhsT(inp["ffn_w1"][li], KT)
        w3 = _tiles_lhsT(inp["ffn_w3"][li], KT)
        w2full = inp["ffn_w2"][li]
    NF = w2full.shape[0] // 128
    NG = NF // FG
    w2 = np.ascontiguousarray(w2full.reshape(NG, FG, 128, KT, 128).transpose(0, 3, 2, 1, 4))
    ones = np.ones((128, 128), f32)
    ident = np.eye(128, dtype=f32)
    maps = []
    for c in range(cfg.NC):
        tsl = slice(c * TC, (c + 1) * TC)
        m = dict(xT=np.ascontiguousarray(xT[:, tsl]), mixin=np.ascontiguousarray(mixall[:, :, tsl]),
                 gluw=gluw, cvec=cvec, wout=wout, w1=w1, w3=w3, w2=w2, ones=ones, ident=ident)
        if moe:
            m["rw"] = np.ascontiguousarray(inp["router_w"][li].reshape(KT, 128, 8).transpose(1, 0, 2))
        maps.append(m)
    return maps


_PROGS = {}


def _prog(key, fn):
    if key not in _PROGS:
        _PROGS[key] = fn()
    return _PROGS[key]


def run_model(inp, cfg, depth=2):
    x = np.asarray(inp["x"], np.float32)
    xT = np.ascontiguousarray(x[0].T)
    cores = list(range(8))
    for layer in range(depth):
        moe = (layer % 2 == 1)
        last = (layer == depth - 1)
        PB = _prog(("B", cfg.D, cfg.S), lambda: build_B(cfg))
        res = run_bass_kernel_spmd(PB.nc, prep_B(inp, layer, cfg, xT), core_ids=cores)
        mixes = [r["mix"] for r in res.results]
        PC = _prog(("C", cfg.D, cfg.S, moe, last), lambda: build_C(cfg, moe, last))
        res = run_bass_kernel_spmd(PC.nc, prep_C(inp, layer, cfg, xT, mixes, moe), core_ids=list(range(cfg.NC)))
        xT = np.concatenate([r["outT"] for r in res.results], axis=1)
    return np.ascontiguousarray(xT.T)[None].astype(np.float32)


def kernel(**inputs):
    inp = {k: np.asarray(v) for k, v in inputs.items()}
    return run_model(inp, Cfg())
```

```python
import contextlib
import math
import numpy as np
import concourse.bass as bass
import concourse.mybir as mybir
from concourse.bass_utils import run_bass_kernel_spmd

F32 = mybir.dt.float32
AF = mybir.ActivationFunctionType
ALU = mybir.AluOpType
ENGS = ["pe", "act", "dve", "pool", "sp"]
EPS = 1e-6


class Cfg:
    D = 4096
    S = 16384
    DFF = 14336
    DFE = 4096
    NE = 8
    NC = 8
    TBB = 256
    TBC = 256
    FG = 16


class Prog:
    def __init__(self):
        self.nc = bass.Bass("TRN2", target_bir_lowering=False)
        self.ops = {e: [] for e in ENGS}
        self.count = {e: 0 for e in ENGS}
        self.waited = {e: {} for e in ENGS}
        self.lastw = {}
        self.readers = {}
        self.dmacnt = {}
        self.n_ops = 0
        self.rr = 0

    def _collect(self, eng, reads, writes):
        need = {}

        def add(tok):
            s, v = tok
            if eng == "pe" and s == "E_pe":
                return
            if need.get(s, 0) < v:
                need[s] = v

        for k in reads:
            t = self.lastw.get(k)
            if t is not None:
                add(t)
        for k in writes:
            t = self.lastw.get(k)
            if t is not None:
                add(t)
            for r in self.readers.get(k, ()):
                add(r)
        out = []
        w = self.waited[eng]
        for s, v in need.items():
            if w.get(s, 0) < v:
                w[s] = v
                out.append((s, v))
        return out

    def _record(self, tok, reads, writes):
        for k in reads:
            self.readers.setdefault(k, []).append(tok)
        for k in writes:
            self.lastw[k] = tok
            self.readers[k] = []

    def op(self, eng, fn, reads=(), writes=()):
        waits = self._collect(eng, reads, writes)
        self.count[eng] += 1
        tok = ("E_" + eng, self.count[eng])
        self.ops[eng].append((waits, fn, "E_" + eng, 1))
        self._record(tok, reads, writes)
        self.n_ops += 1

    def dma(self, out, in_, reads=(), writes=(), sem="dma", eng="sp"):
        waits = self._collect(eng, reads, writes)
        s = "D_" + sem
        self.dmacnt[s] = self.dmacnt.get(s, 0) + 16
        tok = (s, self.dmacnt[s])
        self.ops[eng].append((waits, lambda e: e.dma_start(out=out, in_=in_), s, 16))
        self._record(tok, reads, writes)
        self.n_ops += 1

    @staticmethod
    def K(*aps):
        return [a.tensor.name for a in aps if hasattr(a, "tensor")]

    def act(self, out, in_, func, bias=None, scale=None):
        kw = {}
        if bias is not None:
            kw["bias"] = bias
        if scale is not None:
            kw["scale"] = scale
        self.op("act", lambda e: e.activation(out, in_, func, **kw), self.K(in_, bias, scale), self.K(out))

    def tt(self, eng, out, a, b, op):
        self.op(eng, lambda e: e.tensor_tensor(out, a, b, op), self.K(a, b), self.K(out))

    def ts(self, eng, out, a, s1, s2, op0, op1=None):
        if op1 is None:
            self.op(eng, lambda e: e.tensor_scalar(out, a, s1, None, op0), self.K(a, s1), self.K(out))
        else:
            self.op(eng, lambda e: e.tensor_scalar(out, a, s1, s2, op0, op1), self.K(a, s1, s2), self.K(out))

    def stt(self, eng, out, in0, scalar, in1, op0, op1):
        eng = "dve"
        self.op(eng, lambda e: e.scalar_tensor_tensor(out, in0, scalar, in1, op0, op1),
                self.K(in0, scalar, in1), self.K(out))

    def mm(self, out, lhsT, rhs, start=True, stop=True):
        self.op("pe", lambda e: e.matmul(out, lhsT, rhs, start=start, stop=stop), self.K(lhsT, rhs), self.K(out))

    def tr(self, out, in_, ident):
        self.op("pe", lambda e: e.transpose(out, in_, ident), self.K(in_, ident), self.K(out))

    def copy(self, eng, out, in_):
        if eng == "act":
            self.op("act", lambda e: e.activation(out, in_, AF.Copy), self.K(in_), self.K(out))
        else:
            self.op(eng, lambda e: e.tensor_copy(out, in_), self.K(in_), self.K(out))

    def scan(self, out, d0, d1, init, op0, op1):
        self.op("dve", lambda e: e.tensor_tensor_scan(out, d0, d1, init, op0, op1), self.K(d0, d1, init), self.K(out))

    def recip(self, out, in_):
        self.op("dve", lambda e: e.reciprocal(out, in_), self.K(in_), self.K(out))

    def memset(self, eng, out, val):
        self.op(eng, lambda e: e.memset(out, val), [], self.K(out))

    def load(self, out, in_):
        self.dma(out, in_, writes=self.K(out), sem="L_" + out.tensor.name)

    def store(self, out, in_):
        self.dma(out, in_, reads=self.K(in_), sem="S_" + in_.tensor.name)

    def alt(self):
        self.rr ^= 1
        return "dve" if self.rr else "pool"

    def emit(self):
        nc = self.nc
        names = ["E_" + e for e in ENGS] + sorted(self.dmacnt)
        with contextlib.ExitStack() as st:
            sems = {n: st.enter_context(nc.semaphore(n)) for n in names}
            block = st.enter_context(nc.Block())

            def run(engname):
                def body(e):
                    for waits, fn, sname, inc in self.ops[engname]:
                        for s, v in waits:
                            e.wait_ge(sems[s], v)
                        fn(e).then_inc(sems[sname], inc)
                    if engname == "sp":
                        for s in sorted(self.dmacnt):
                            e.wait_ge(sems[s], self.dmacnt[s])
                        for en in ENGS:
                            if en != "sp" and self.count[en] > 0:
                                e.wait_ge(sems["E_" + en], self.count[en])
                return body

            block.sync(run("sp"))
            block.tensor(run("pe"))
            block.scalar(run("act"))
            block.vector(run("dve"))
            block.gpsimd(run("pool"))
        return nc


def gelu_tanh(P, st_alloc, x, out, tmp1, tmp2):
    P.tt("pool", tmp1, x, x, ALU.mult)
    P.ts("dve", tmp1, tmp1, 0.044715, 1.0, ALU.mult, ALU.add)
    P.tt("pool", tmp1, tmp1, x, ALU.mult)
    P.act(tmp2, tmp1, AF.Sigmoid, scale=1.5957691216057308)
    P.tt("dve", out, x, tmp2, ALU.mult)


ZQ, ZF, ZI, ZG, ZU, RQ0, RQ1, RK0, RK1, RV, RG_, GG, GX = range(13)


def build_B(cfg):
    D, S, TB = cfg.D, cfg.S, cfg.TBB
    KT = D // 128
    NBLK = S // TB
    NCH = TB // 64
    P = Prog()
    nc = P.nc

    def din(name, shape):
        return nc.dram_tensor(name, shape, F32, kind="ExternalInput").ap()

    xT = din("xT", [D, S])
    gmix = din("gmix", [128, KT])
    win = din("win", [13, 128, KT, 128])
    ones_d = din("ones", [128, 128])
    ident_d = din("ident", [128, 128])
    maskT_d = din("maskT", [64, 64])
    hgp = din("hgp", [128, 4])
    s5s = din("s5s", [128, 12])
    s5bd = din("s5bd", [7, 4, 128, 128])
    s5d = din("s5d", [128, 1])
    cos_d = din("cosT", [128, S])
    sin_d = din("sinT", [128, S])
    retc = din("retc", [128, 2 * TB + 1])
    dmat_d = din("dmatT", [64, 64])
    rgp = din("rgp", [128, 8])
    rgw = din("rgw", [2, 128, 128])
    mix = nc.dram_tensor("mix", [5, 128, S], F32, kind="ExternalOutput").ap()

    with contextlib.ExitStack() as st:
        def sb(name, shape):
            return st.enter_context(nc.sbuf_tensor(name, shape, F32))

        def pb(name):
            return st.enter_context(nc.psum_tensor(name, [128, TB], F32))

        ones = sb("ones_s", [128, 128]); P.load(ones[:, :], ones_d)
        ident = sb("ident_s", [128, 128]); P.load(ident[:, :], ident_d)
        maskT = sb("maskT_s", [64, 64]); P.load(maskT[:, :], maskT_d)
        dmatT = sb("dmatT_s", [64, 64]); P.load(dmatT[:, :], dmat_d)
        gm = sb("gm_s", [128, KT]); P.load(gm[:, :], gmix)
        hg = sb("hg_s", [128, 4]); P.load(hg[:, :], hgp)
        s5st = sb("s5st", [128, 12]); P.load(s5st[:, :], s5s)
        s5dd = sb("s5dd", [128, 1]); P.load(s5dd[:, :], s5d)
        rc = sb("retc_s", [128, 2 * TB + 1]); P.load(rc[:, :], retc)
        rg = sb("rgp_s", [128, 8]); P.load(rg[:, :], rgp)
        wa = sb("wa_s", [128, 128]); P.load(wa[:, :], rgw[0])
        wx = sb("wx_s", [128, 128]); P.load(wx[:, :], rgw[1])
        onesT = sb("onesT", [128, TB]); P.memset("pool", onesT[:, :], 1.0)

        pss = pb("pss"); pz = [pb("pz0"), pb("pz1")]
        pm = [pb(f"pm{i}") for i in range(5)]

        hgc = sb("hgc", [128, 4])
        P.tt("dve", hgc[:, 0:1], hg[:, 1:2], hg[:, 0:1], ALU.subtract)
        P.act(hgc[:, 1:2], hgc[:, 0:1], AF.Sigmoid)
        P.tt("dve", hgc[:, 2:3], hgc[:, 1:2], hg[:, 2:3], ALU.mult)
        P.ts("dve", hgc[:, 3:4], hgc[:, 2:3], -1.0, 1.0, ALU.mult, ALU.add)
        lb_c, oml_c, hng_c = hgc[:, 2:3], hgc[:, 3:4], hg[:, 3:4]

        rgc = sb("rgc", [128, 2])
        P.act(rgc[:, 0:1], rg[:, 7:8], AF.Exp, scale=-1.0)
        P.act(rgc[:, 1:2], rgc[:, 0:1], AF.Ln, bias=1.0)
        P.ts("dve", rgc[:, 1:2], rgc[:, 1:2], -8.0, None, ALU.mult)
        rg_c = rgc[:, 1:2]

        TWO_PI = 2.0 * math.pi

        _dcache = {}

        def s5_disc(tag, lre, lim, ldt, F, want_coef):
            if tag not in _dcache:
                _dcache[tag] = ({n: sb(f"{tag}_{n}", [128, F]) for n in
                                 ["dt", "mag", "ang", "r", "cos", "sin", "abr", "abi", "den", "nr", "t1", "t2", "cr", "ci"]},
                                st.enter_context(nc.sbuf_tensor(f"{tag}_int", [128, F], mybir.dt.int32)))
            t, ti = _dcache[tag]
            P.act(t["dt"][:, :], ldt, AF.Exp)
            P.tt("dve", t["mag"][:, :], lre, t["dt"][:, :], ALU.mult)
            P.act(t["mag"][:, :], t["mag"][:, :], AF.Exp)
            P.tt("dve", t["ang"][:, :], lim, t["dt"][:, :], ALU.mult)
            for (dst, shift) in (("sin", 0.0), ("cos", 0.5 * math.pi)):
                P.ts("dve", t["t1"][:, :], t["ang"][:, :], shift, None, ALU.add)
                P.ts("dve", t["r"][:, :], t["t1"][:, :], 1.0 / TWO_PI, None, ALU.mult)
                P.copy("dve", ti[:, :], t["r"][:, :])
                P.copy("dve", t["r"][:, :], ti[:, :])
                P.stt("dve", t["r"][:, :], t["r"][:, :], -TWO_PI, t["t1"][:, :], ALU.mult, ALU.add)
                P.act(t[dst][:, :], t["r"][:, :], AF.Sin)
            P.tt("dve", t["abr"][:, :], t["mag"][:, :], t["cos"][:, :], ALU.mult)
            P.tt("dve", t["abi"][:, :], t["mag"][:, :], t["sin"][:, :], ALU.mult)
            if want_coef:
                P.tt("dve", t["den"][:, :], lre, lre, ALU.mult)
                P.tt("dve", t["t1"][:, :], lim, lim, ALU.mult)
                P.tt("dve", t["den"][:, :], t["den"][:, :], t["t1"][:, :], ALU.add)
                P.recip(t["den"][:, :], t["den"][:, :])
                P.ts("dve", t["nr"][:, :], t["abr"][:, :], -1.0, None, ALU.add)
                P.tt("dve", t["t1"][:, :], t["nr"][:, :], lre, ALU.mult)
                P.tt("dve", t["t2"][:, :], t["abi"][:, :], lim, ALU.mult)
                P.tt("dve", t["t1"][:, :], t["t1"][:, :], t["t2"][:, :], ALU.add)
                P.tt("dve", t["cr"][:, :], t["t1"][:, :], t["den"][:, :], ALU.mult)
                P.tt("dve", t["t1"][:, :], t["abi"][:, :], lre, ALU.mult)
                P.tt("dve", t["t2"][:, :], t["nr"][:, :], lim, ALU.mult)
                P.tt("dve", t["t1"][:, :], t["t1"][:, :], t["t2"][:, :], ALU.subtract)
                P.tt("dve", t["ci"][:, :], t["t1"][:, :], t["den"][:, :], ALU.mult)
            return t

        ds_ = s5_disc("s5a", s5st[:, 0:4], s5st[:, 4:8], s5st[:, 8:12], 4, False)
        NSTEP = int(math.log2(TB))
        pw = [sb(f"s5pw{k}", [128, 12]) for k in range(NSTEP)]
        P.copy("dve", pw[0][:, 0:4], ds_["abr"][:, :])
        P.copy("dve", pw[0][:, 4:8], ds_["abi"][:, :])
        P.ts("dve", pw[0][:, 8:12], ds_["abi"][:, :], -1.0, None, ALU.mult)
        s5tmp = sb("s5tmp", [128, 8])
        for k in range(1, NSTEP):
            a, b = pw[k - 1][:, 0:4], pw[k - 1][:, 4:8]
            P.tt("dve", s5tmp[:, 0:4], a, a, ALU.mult)
            P.tt("dve", s5tmp[:, 4:8], b, b, ALU.mult)
            P.tt("dve", pw[k][:, 0:4], s5tmp[:, 0:4], s5tmp[:, 4:8], ALU.subtract)
            P.tt("dve", s5tmp[:, 0:4], a, b, ALU.mult)
            P.ts("dve", pw[k][:, 4:8], s5tmp[:, 0:4], 2.0, None, ALU.mult)
            P.ts("dve", pw[k][:, 8:12], s5tmp[:, 0:4], -2.0, None, ALU.mult)
        BDre, BDim, CDre, CDimn = [], [], [], []
        for j in range(4):
            if j == 0:
                ldtmp = [sb(f"s5ldt_{i}", [128, 128]) for i in range(5)]
            ld = ldtmp + [sb(f"s5ld{j}_{i}", [128, 128]) for i in (5, 6)]
            for i in range(7):
                P.load(ld[i][:, :], s5bd[i, j])
            dd = s5_disc("s5b", ld[0][:, :], ld[1][:, :], ld[2][:, :], 128, True)
            bre = sb(f"BDre{j}", [128, 128]); bim = sb(f"BDim{j}", [128, 128])
            t1 = dd["t1"][:, :]; t2 = dd["t2"][:, :]
            P.tt("dve", t1, dd["cr"][:, :], ld[3][:, :], ALU.mult)
            P.tt("dve", t2, dd["ci"][:, :], ld[4][:, :], ALU.mult)
            P.tt("dve", bre[:, :], t1, t2, ALU.subtract)
            P.tt("dve", t1, dd["cr"][:, :], ld[4][:, :], ALU.mult)
            P.tt("dve", t2, dd["ci"][:, :], ld[3][:, :], ALU.mult)
            P.tt("dve", bim[:, :], t1, t2, ALU.add)
            P.ts("dve", ld[6][:, :], ld[6][:, :], -1.0, None, ALU.mult)
            BDre.append(bre); BDim.append(bim); CDre.append(ld[5]); CDimn.append(ld[6])

        xpad = sb("xpad", [128, TB + 3]); P.memset("pool", xpad[:, :], 0.0)
        hlast = sb("hlast", [128, 1]); P.memset("pool", hlast[:, :], 0.0)
        s5car = sb("s5car", [128, 8]); P.memset("pool", s5car[:, :], 0.0)
        Sh = sb("Sh", [128, 128]); P.memset("pool", Sh[:, :], 0.0)
        Sa = sb("Sa", [128, 128]); P.memset("pool", Sa[:, :], 0.0)
        Sb = sb("Sb", [128, 128]); P.memset("pool", Sb[:, :], 0.0)

        xsb = [sb("xs0", [128, KT, TB]), sb("xs1", [128, KT, TB])]
        sq = [sb("sq0", [128, TB]), sb("sq1", [128, TB])]
        rstdb = [sb("rstd0", [128, TB]), sb("rstd1", [128, TB])]
        wt = [sb("wt0", [128, KT, 128]), sb("wt1", [128, KT, 128])]
        zsb = [[sb(f"z{b}_{i}", [128, TB]) for i in range(13)] for b in range(2)]
        W = [sb(f"w{i}", [128, TB]) for i in range(13)]
        s5b = [[sb(f"s5x{j}_{i}", [128, TB]) for i in range(4)] for j in range(4)]
        small = [sb(f"sm{i}", [64, 128]) for i in range(6)]
        scT = sb("scT", [64, 64])
        cosb = sb("cosb", [128, TB]); sinb = sb("sinb", [128, TB])
        nmid = sb("nmid", [128, NCH])
        ob = [sb(f"ob{i}", [128, TB]) for i in range(5)]

        wcnt = [0]

        def load_norm(blk):
            xs = xsb[blk % 2]; rstd = rstdb[blk % 2]
            tsl = slice(blk * TB, (blk + 1) * TB)
            P.load(xs[:, :, :], xT[:, tsl].rearrange("(kt p) t -> p kt t", p=128))
            for kt in range(KT):
                s_ = sq[kt % 2]
                P.tt("pool", s_[:, :], xs[:, kt, :], xs[:, kt, :], ALU.mult)
                P.mm(pss[:, :], ones[:, :], s_[:, :], start=(kt == 0), stop=(kt == KT - 1))
            P.act(rstd[:, :], pss[:, :], AF.Sqrt, bias=EPS, scale=1.0 / D)
            P.recip(rstd[:, :], rstd[:, :])
            for kt in range(KT):
                P.stt("dve", xs[:, kt, :], xs[:, kt, :], gm[:, kt:kt + 1], rstd[:, :], ALU.mult, ALU.mult)

        def inproj_tasks(blk):
            xs = xsb[blk % 2]; zz = zsb[blk % 2]
            for ct in range(13):
                b = wcnt[0] % 2; wcnt[0] += 1
                P.load(wt[b][:, :, :], win[ct])
                for kt in range(KT):
                    P.mm(pz[b][:, :], wt[b][:, kt, :], xs[:, kt, :], start=(kt == 0), stop=(kt == KT - 1))
                P.copy("act", zz[ct][:, :], pz[b][:, :])
                yield ct

        def step(gen, n):
            if gen is None:
                return
            for _ in range(n):
                next(gen, None)

        load_norm(0)
        for _ in inproj_tasks(0):
            pass
        for blk in range(NBLK):
            ts_ = slice(blk * TB, (blk + 1) * TB)
            zs = zsb[blk % 2]
            gen = None
            if blk + 1 < NBLK:
                load_norm(blk + 1)
                gen = inproj_tasks(blk + 1)
            step(gen, 2)

            G, X = zs[GG], zs[GX]
            xc, r_, i_, a_, t1, t2 = W[0], W[1], W[2], W[3], W[4], W[5]
            P.copy("pool", xpad[:, 3:TB + 3], X[:, :])
            P.ts("dve", xc[:, :], xpad[:, 0:TB], rg[:, 0:1], rg[:, 4:5], ALU.mult, ALU.add)
            for tap in range(1, 4):
                P.stt("dve", xc[:, :], xpad[:, tap:tap + TB], rg[:, tap:tap + 1], xc[:, :], ALU.mult, ALU.add)
            P.copy("pool", xpad[:, 0:3], xpad[:, TB:TB + 3])
            P.mm(pm[0][:, :], wa[:, :], xc[:, :])
            P.act(r_[:, :], pm[0][:, :], AF.Sigmoid, bias=rg[:, 5:6])
            P.mm(pm[1][:, :], wx[:, :], xc[:, :])
            P.act(i_[:, :], pm[1][:, :], AF.Sigmoid, bias=rg[:, 6:7])
            P.act(a_[:, :], r_[:, :], AF.Exp, scale=rg_c)
            P.tt("pool", t1[:, :], a_[:, :], a_[:, :], ALU.mult)
            P.act(t1[:, :], t1[:, :], AF.Sqrt, bias=1.0, scale=-1.0)
            P.tt("pool", t2[:, :], i_[:, :], xc[:, :], ALU.mult)
            P.tt("pool", t2[:, :], t2[:, :], t1[:, :], ALU.mult)
            P.scan(r_[:, :], a_[:, :], t2[:, :], hlast[:, 0:1], ALU.mult, ALU.add)
            P.copy("act", hlast[:, 0:1], r_[:, TB - 1:TB])
            gelu_tanh(P, None, G[:, :], i_[:, :], t1[:, :], t2[:, :])
            P.tt("dve", ob[4][:, :], i_[:, :], r_[:, :], ALU.mult)
            P.store(mix[4, :, ts_], ob[4][:, :])

            step(gen, 2)
            U = zs[ZU]
            fin = []
            for j in range(4):
                eng = "dve" if j % 2 == 0 else "pool"
                Ar, Ai, Br, Bi = s5b[j]
                P.mm(pm[0 + (j % 2) * 2][:, :], BDre[j][:, :], U[:, :])
                P.copy("act", Ar[:, :], pm[0 + (j % 2) * 2][:, :])
                P.mm(pm[1 + (j % 2) * 2][:, :], BDim[j][:, :], U[:, :])
                P.copy("act", Ai[:, :], pm[1 + (j % 2) * 2][:, :])
                abr, abi, nabi = pw[0][:, j:j + 1], pw[0][:, 4 + j:5 + j], pw[0][:, 8 + j:9 + j]
                cr, ci = s5car[:, j:j + 1], s5car[:, 4 + j:5 + j]
                P.stt(eng, Ar[:, 0:1], cr, abr, Ar[:, 0:1], ALU.mult, ALU.add)
                P.stt(eng, Ar[:, 0:1], ci, nabi, Ar[:, 0:1], ALU.mult, ALU.add)
                P.stt(eng, Ai[:, 0:1], ci, abr, Ai[:, 0:1], ALU.mult, ALU.add)
                P.stt(eng, Ai[:, 0:1], cr, abi, Ai[:, 0:1], ALU.mult, ALU.add)
                cur, nxt = (Ar, Ai), (Br, Bi)
                for k in range(NSTEP):
                    d = 1 << k
                    pr, pi, npi = pw[k][:, j:j + 1], pw[k][:, 4 + j:5 + j], pw[k][:, 8 + j:9 + j]
                    cr_, ci_ = cur
                    nr_, ni_ = nxt
                    P.stt(eng, nr_[:, d:TB], cr_[:, 0:TB - d], pr, cr_[:, d:TB], ALU.mult, ALU.add)
                    P.stt(eng, nr_[:, d:TB], ci_[:, 0:TB - d], npi, nr_[:, d:TB], ALU.mult, ALU.add)
                    P.stt(eng, ni_[:, d:TB], ci_[:, 0:TB - d], pr, ci_[:, d:TB], ALU.mult, ALU.add)
                    P.stt(eng, ni_[:, d:TB], cr_[:, 0:TB - d], pi, ni_[:, d:TB], ALU.mult, ALU.add)
                    P.copy("act", nr_[:, 0:d], cr_[:, 0:d])
                    P.copy("act", ni_[:, 0:d], ci_[:, 0:d])
                    cur, nxt = nxt, cur
                fin.append(cur)
                P.copy("act", s5car[:, j:j + 1], cur[0][:, TB - 1:TB])
                P.copy("act", s5car[:, 4 + j:5 + j], cur[1][:, TB - 1:TB])
            for j in range(4):
                P.mm(pm[4][:, :], CDre[j][:, :], fin[j][0][:, :], start=(j == 0), stop=False)
                P.mm(pm[4][:, :], CDimn[j][:, :], fin[j][1][:, :], start=False, stop=(j == 3))
            y2, t1, t2 = W[0], W[1], W[2]
            P.stt("dve", y2[:, :], U[:, :], s5dd[:, 0:1], pm[4][:, :], ALU.mult, ALU.add)
            gelu_tanh(P, None, y2[:, :], ob[1][:, :], t1[:, :], t2[:, :])
            P.store(mix[1, :, ts_], ob[1][:, :])

            step(gen, 3)
            Q, Fz, I, G = zs[ZQ], zs[ZF], zs[ZI], zs[ZG]
            qs, f_, lf, k_, cum, e1, e2, e3, ecum = W[3], W[4], W[5], W[6], W[7], W[8], W[9], W[10], W[11]
            P.act(qs[:, :], Q[:, :], AF.Silu)
            P.act(f_[:, :], Fz[:, :], AF.Sigmoid)
            P.ts("dve", f_[:, :], f_[:, :], oml_c, lb_c, ALU.mult, ALU.add)
            P.act(lf[:, :], f_[:, :], AF.Ln)
            P.ts("pool", k_[:, :], f_[:, :], -1.0, 1.0, ALU.mult, ALU.add)
            for c in range(NCH):
                cs = slice(c * 64, (c + 1) * 64)
                P.scan(cum[:, cs], onesT[:, 0:64], lf[:, cs], 0.0, ALU.mult, ALU.add)
            cum3 = cum[:, :].rearrange("p (c l) -> p c l", l=64)
            P.ts("dve", nmid[:, :], cum3[:, :, 31], -1.0, None, ALU.mult)
            for c in range(NCH):
                cs = slice(c * 64, (c + 1) * 64)
                mid = cum[:, c * 64 + 31:c * 64 + 32]
                last = cum[:, c * 64 + 63:c * 64 + 64]
                P.act(e1[:, cs], cum[:, cs], AF.Exp, bias=nmid[:, c:c + 1], scale=1.0)
                P.act(e2[:, cs], cum[:, cs], AF.Exp, bias=mid, scale=-1.0)
                P.act(e3[:, cs], cum[:, cs], AF.Exp, bias=last, scale=-1.0)
            P.act(ecum[:, :], cum[:, :], AF.Exp)
            P.tt("dve", e1[:, :], e1[:, :], qs[:, :], ALU.mult)
            P.tt("pool", e2[:, :], e2[:, :], k_[:, :], ALU.mult)
            P.tt("pool", e3[:, :], e3[:, :], k_[:, :], ALU.mult)
            P.tt("dve", qs[:, :], qs[:, :], ecum[:, :], ALU.mult)
            for c in range(NCH):
                cs = slice(c * 64, (c + 1) * 64)
                Kun, Vn = small[0], small[1]
                P.tr(pm[0][0:64, 0:128], e3[:, cs], ident[:, :])
                P.copy("act", Kun[:, :], pm[0][0:64, 0:128])
                P.tr(pm[1][0:64, 0:128], I[:, cs], ident[:, :])
                P.copy("act", Vn[:, :], pm[1][0:64, 0:128])
                P.mm(pm[2][0:64, 0:64], e2[:, cs], e1[:, cs])
                P.tt("dve", scT[:, :], pm[2][0:64, 0:64], maskT[:, :], ALU.mult)
                P.mm(pm[4][:, cs], Vn[:, :], scT[:, :], start=True, stop=False)
                P.mm(pm[4][:, cs], Sh[:, :], qs[:, cs], start=False, stop=True)
                P.mm(pm[3][:, 0:128], Kun[:, :], Vn[:, :])
                P.stt("dve", Sh[:, :], Sh[:, :], ecum[:, c * 64 + 63:c * 64 + 64], pm[3][:, 0:128], ALU.mult, ALU.add)
            osb, t1, t2 = W[0], W[1], W[2]
            P.copy("act", osb[:, :], pm[4][:, :])
            P.tt("pool", t1[:, :], osb[:, :], osb[:, :], ALU.mult)
            P.mm(pm[0][:, :], ones[:, :], t1[:, :])
            P.act(t2[:, :], pm[0][:, :], AF.Sqrt, bias=EPS, scale=1.0 / 128.0)
            P.recip(t2[:, :], t2[:, :])
            P.stt("dve", osb[:, :], osb[:, :], hng_c, t2[:, :], ALU.mult, ALU.mult)
            P.act(t1[:, :], G[:, :], AF.Silu)
            P.tt("dve", ob[0][:, :], osb[:, :], t1[:, :], ALU.mult)
            P.store(mix[0, :, ts_], ob[0][:, :])

            step(gen, 3)
            P.load(cosb[:, :], cos_d[:, ts_])
            P.load(sinb[:, :], sin_d[:, ts_])
            qa, qb, ka, kb, t1, t2 = W[3], W[4], W[5], W[6], W[7], W[8]
            for (x0, x1, oa, ob_) in ((zs[RQ0], zs[RQ1], qa, qb), (zs[RK0], zs[RK1], ka, kb)):
                P.tt("dve", t1[:, :], x0[:, :], cosb[:, :], ALU.mult)
                P.tt("pool", t2[:, :], x1[:, :], sinb[:, :], ALU.mult)
                P.tt("dve", oa[:, :], t1[:, :], t2[:, :], ALU.subtract)
                P.tt("pool", t1[:, :], x0[:, :], sinb[:, :], ALU.mult)
                P.tt("dve", t2[:, :], x1[:, :], cosb[:, :], ALU.mult)
                P.tt("pool", ob_[:, :], t1[:, :], t2[:, :], ALU.add)
            qia, qib, kua, kub = W[9], W[10], W[11], W[12]
            P.tt("dve", qia[:, :], qa[:, :], rc[:, 0:TB], ALU.mult)
            P.tt("pool", qib[:, :], qb[:, :], rc[:, 0:TB], ALU.mult)
            P.tt("dve", kua[:, :], ka[:, :], rc[:, TB:2 * TB], ALU.mult)
            P.tt("pool", kub[:, :], kb[:, :], rc[:, TB:2 * TB], ALU.mult)
            cdec = rc[:, 2 * TB:2 * TB + 1]
            Vz = zs[RV]
            for c in range(NCH):
                cs = slice(c * 64, (c + 1) * 64)
                Kna, Knb, Vn = small[2], small[3], small[4]
                P.tr(pm[0][0:64, 0:128], kua[:, cs], ident[:, :])
                P.copy("act", Kna[:, :], pm[0][0:64, 0:128])
                P.tr(pm[1][0:64, 0:128], kub[:, cs], ident[:, :])
                P.copy("act", Knb[:, :], pm[1][0:64, 0:128])
                P.tr(pm[2][0:64, 0:128], Vz[:, cs], ident[:, :])
                P.copy("act", Vn[:, :], pm[2][0:64, 0:128])
                P.mm(pm[3][0:64, 0:64], ka[:, cs], qa[:, cs], start=True, stop=False)
                P.mm(pm[3][0:64, 0:64], kb[:, cs], qb[:, cs], start=False, stop=True)
                P.tt("dve", scT[:, :], pm[3][0:64, 0:64], dmatT[:, :], ALU.mult)
                P.mm(pm[4][:, cs], Vn[:, :], scT[:, :], start=True, stop=False)
                P.mm(pm[4][:, cs], Sa[:, :], qia[:, cs], start=False, stop=False)
                P.mm(pm[4][:, cs], Sb[:, :], qib[:, cs], start=False, stop=True)
                P.mm(pm[0][:, 0:128], Kna[:, :], Vn[:, :])
                P.stt("dve", Sa[:, :], Sa[:, :], cdec, pm[0][:, 0:128], ALU.mult, ALU.add)
                P.mm(pm[1][:, 0:128], Knb[:, :], Vn[:, :])
                P.stt("dve", Sb[:, :], Sb[:, :], cdec, pm[1][:, 0:128], ALU.mult, ALU.add)
            P.copy("act", ob[2][:, :], pm[4][:, :])
            P.store(mix[2, :, ts_], ob[2][:, :])
            P.act(ob[3][:, :], zs[RG_][:, :], AF.Silu)
            P.store(mix[3, :, ts_], ob[3][:, :])
            step(gen, 13)
        P.emit()
    return P


def _tiles_lhsT(w, KT):
    K, M = w.shape
    return np.ascontiguousarray(w.reshape(KT, 128, M // 128, 128).transpose(2, 1, 0, 3))


def _ret_consts(h, TB):
    f32 = np.float32
    lg = np.log(f32(1.0) - f32(2.0) ** (f32(-5.0) - f32(h))).astype(f32)
    idx = np.arange(64, dtype=f32)
    rel = idx[None, :] - idx[:, None]
    dmatT = np.where(rel >= 0, np.exp(np.maximum(rel, 0) * lg), 0.0).astype(f32) * f32(256 ** -0.5)
    qdec = np.exp((idx + 1.0) * lg).astype(f32)
    kdec = (np.exp((63.0 - idx) * lg) * f32(256 ** -0.5)).astype(f32)
    cdec = np.exp(f32(64.0) * lg).astype(f32)
    rc = np.zeros((128, 2 * TB + 1), f32)
    rc[:, 0:TB] = np.tile(qdec, TB // 64)[None, :]
    rc[:, TB:2 * TB] = np.tile(kdec, TB // 64)[None, :]
    rc[:, 2 * TB] = cdec
    return dmatT.astype(f32), rc


def prep_B(inp, layer, cfg, xT):
    D, S = cfg.D, cfg.S
    KT = D // 128
    f32 = np.float32
    w_in = inp["w_in"][layer]
    ones = np.ones((128, 128), f32)
    ident = np.eye(128, dtype=f32)
    maskT = np.triu(np.ones((64, 64), f32))
    pos = np.arange(S, dtype=f32)
    inv_freq = (f32(10000.0) ** (-np.arange(0, 256, 2, dtype=f32) / f32(256))).astype(f32)
    ang = (pos[None, :] * inv_freq[:, None]).astype(f32)
    cosT = np.cos(ang).astype(f32)
    sinT = np.sin(ang).astype(f32)
    gmix = np.ascontiguousarray(inp["norm_mix_g"][layer].reshape(KT, 128).T)
    maps = []
    for c in range(8):
        h, half = c // 2, c % 2
        cols = [0 + c * 128, 1024 + c * 128, 2048 + c * 128, 3072 + c * 128, 4096 + c * 128,
                5120 + h * 256, 5120 + h * 256 + 128, 6144 + h * 256, 6144 + h * 256 + 128,
                7168 + h * 256 + half * 128, 8192 + h * 256 + half * 128,
                9216 + c * 128, 10240 + c * 128]
        wsel = np.concatenate([w_in[:, c0:c0 + 128] for c0 in cols], axis=1)
        win = _tiles_lhsT(wsel, KT)
        ch = slice(c * 128, (c + 1) * 128)
        hgp = np.stack([inp["hgrn_lb_logits"][0, ch], inp["hgrn_lb_logits"][1, ch],
                        np.full(128, float(layer), f32), inp["hgrn_norm_g"][layer, ch]], axis=1).astype(f32)
        s5s = np.zeros((128, 12), f32)
        s5bd = np.zeros((7, 4, 128, 128), f32)
        for j in range(4):
            for gl in range(2):
                gg = 2 * j + gl
                g = 8 * c + gg
                ps = slice(gl * 64, gl * 64 + 64)
                s5s[ps, j] = inp["s5_lambda_re"][layer, g]
                s5s[ps, 4 + j] = inp["s5_lambda_im"][layer, g]
                s5s[ps, 8 + j] = inp["s5_log_dt"][layer, g]
                s5bd[0, j, :, ps] = inp["s5_lambda_re"][layer, g][None, :]
                s5bd[1, j, :, ps] = inp["s5_lambda_im"][layer, g][None, :]
                s5bd[2, j, :, ps] = inp["s5_log_dt"][layer, g]
                rs = slice(gg * 16, gg * 16 + 16)
                s5bd[3, j, rs, ps] = inp["s5_b_re"][layer, g].T
                s5bd[4, j, rs, ps] = inp["s5_b_im"][layer, g].T
                s5bd[5, j, ps, rs] = inp["s5_c_re"][layer, g].T
                s5bd[6, j, ps, rs] = inp["s5_c_im"][layer, g].T
        s5d = np.ascontiguousarray(inp["s5_d"][layer, ch].reshape(128, 1))
        dmatT, rc = _ret_consts(h, cfg.TBB)
        rgp = np.stack([inp["rg_conv_w"][layer, 0, ch], inp["rg_conv_w"][layer, 1, ch],
                        inp["rg_conv_w"][layer, 2, ch], inp["rg_conv_w"][layer, 3, ch],
                        inp["rg_conv_b"][layer, ch], inp["rg_b_a"][layer, ch],
                        inp["rg_b_x"][layer, ch], inp["rg_lambda"][layer, ch]], axis=1).astype(f32)
        rgw = np.stack([inp["rg_w_a"][layer, c], inp["rg_w_x"][layer, c]]).astype(f32)
        maps.append(dict(xT=xT, gmix=gmix, win=win, ones=ones, ident=ident, maskT=maskT, hgp=hgp,
                         s5s=s5s, s5bd=s5bd, s5d=s5d, cosT=cosT, sinT=sinT, retc=rc, dmatT=dmatT,
                         rgp=rgp, rgw=rgw))
    return maps


def build_C(cfg, moe, last):
    D, TB, FG = cfg.D, cfg.TBC, cfg.FG
    TC = cfg.S // cfg.NC
    KT = D // 128
    DT = KT
    NBLK = TC // TB
    NF = (cfg.NE * cfg.DFE if moe else cfg.DFF) // 128
    NG = NF // FG
    FPE = cfg.DFE // 128
    P = Prog()
    nc = P.nc

    def din(name, shape):
        return nc.dram_tensor(name, shape, F32, kind="ExternalInput").ap()

    xT = din("xT", [D, TC])
    mixin = din("mixin", [40, 128, TC])
    gluw = din("gluw", [8, 128, 8, 128])
    cvec = din("cvec", [128, 16 + 2 * KT])
    wout = din("wout", [DT, 128, 32, 128])
    w1 = din("w1", [NF, 128, KT, 128])
    w3 = din("w3", [NF, 128, KT, 128])
    w2 = din("w2", [NG, DT, 128, FG, 128])
    ones_d = din("ones", [128, 128])
    ident_d = din("ident", [128, 128])
    if moe:
        rw = din("rw", [128, KT, 8])
    outT = nc.dram_tensor("outT", [D, TC], F32, kind="ExternalOutput").ap()

    with contextlib.ExitStack() as st:
        def sb(name, shape):
            return st.enter_context(nc.sbuf_tensor(name, shape, F32))

        def pb(name):
            return st.enter_context(nc.psum_tensor(name, [128, TB], F32))

        ones = sb("ones_s", [128, 128]); P.load(ones[:, :], ones_d)
        ident = sb("ident_s", [128, 128]); P.load(ident[:, :], ident_d)
        cv = sb("cv_s", [128, 16 + 2 * KT]); P.load(cv[:, :], cvec)
        if moe:
            rws = sb("rw_s", [128, KT, 8]); P.load(rws[:, :, :], rw)
        pss = pb("pss"); pz = [pb("pz0"), pb("pz1")]
        pa = [pb("pa0"), pb("pa1")]; pbb = [pb("pb0"), pb("pb1")]; pr = pb("pr")

        xs = sb("xs", [128, KT, TB])
        hs = sb("hs", [128, max(KT, 24), TB])
        mixs = sb("mixs", [128, 32, TB])
        wtA = [sb("wtA0", [128, max(KT, 32), 128]), sb("wtA1", [128, max(KT, 32), 128])]
        wtB = [sb("wtB0", [128, KT, 128]), sb("wtB1", [128, KT, 128])]
        w2b = [sb("w2b0", [128, FG, 128]), sb("w2b1", [128, FG, 128])]
        sq = [sb("sq0", [128, TB]), sb("sq1", [128, TB])]
        rstd = sb("rstd", [128, TB])
        T = [sb(f"t{i}", [128, TB]) for i in range(6)]
        sil = [sb("sil0", [128, TB]), sb("sil1", [128, TB])]
        if moe:
            cb = [sb(f"cb{e}", [128, TB]) for e in range(8)]
            lg = sb("lg", [128, 8]); l2 = sb("l2", [128, 8]); eq1 = sb("eq1", [128, 8]); eq2 = sb("eq2", [128, 8])
            comb = sb("comb", [128, 8]); mm_ = sb("mm_", [128, 4]); rep = sb("rep", [128, 128])

        def rmsnorm(src, dst, gcol0):
            for kt in range(KT):
                s = sq[kt % 2]
                P.act(s[:, :], src[:, kt, :], AF.Square)
                P.mm(pss[:, :], ones[:, :], s[:, :], start=(kt == 0), stop=(kt == KT - 1))
            P.act(rstd[:, :], pss[:, :], AF.Sqrt, bias=EPS, scale=1.0 / D)
            P.recip(rstd[:, :], rstd[:, :])
            for kt in range(KT):
                P.stt("dve", dst[:, kt, :], src[:, kt, :], cv[:, gcol0 + kt:gcol0 + kt + 1], rstd[:, :], ALU.mult, ALU.mult)

        wi = 0
        for blk in range(NBLK):
            ts_ = slice(blk * TB, (blk + 1) * TB)
            P.load(xs[:, :, :], xT[:, ts_].rearrange("(kt p) t -> p kt t", p=128))
            P.load(mixs[:, 0:8, :], mixin[0:8, :, ts_].rearrange("k p t -> p k t"))
            P.load(mixs[:, 24:32, :], mixin[32:40, :, ts_].rearrange("k p t -> p k t"))
            ys, ro, rgt = hs[:, 0:8, :], hs[:, 8:16, :], hs[:, 16:24, :]
            P.load(hs[:, 0:24, :], mixin[8:32, :, ts_].rearrange("k p t -> p k t"))
            for jt in range(8):
                b = wi % 2; wi += 1
                P.load(wtA[b][:, 0:8, :], gluw[jt])
                for it in range(8):
                    P.mm(pz[b][:, :], wtA[b][:, it, :], hs[:, it, :], start=(it == 0), stop=(it == 7))
                P.act(T[0][:, :], pz[b][:, :], AF.Sigmoid, bias=cv[:, jt:jt + 1])
                P.tt("dve", mixs[:, 8 + jt, :], hs[:, jt, :], T[0][:, :], ALU.mult)
            for h in range(4):
                t0, t1 = 8 + 2 * h, 8 + 2 * h + 1
                P.act(sq[0][:, :], hs[:, t0, :], AF.Square)
                P.act(sq[1][:, :], hs[:, t1, :], AF.Square)
                P.mm(pa[0][:, :], ones[:, :], hs[:, t0, :], start=True, stop=False)
                P.mm(pa[0][:, :], ones[:, :], hs[:, t1, :], start=False, stop=True)
                P.mm(pbb[0][:, :], ones[:, :], sq[0][:, :], start=True, stop=False)
                P.mm(pbb[0][:, :], ones[:, :], sq[1][:, :], start=False, stop=True)
                mean, msq, var = T[1], T[2], T[3]
                P.act(mean[:, :], pa[0][:, :], AF.Copy, scale=1.0 / 256.0)
                P.tt("pool", msq[:, :], mean[:, :], mean[:, :], ALU.mult)
                P.stt("dve", var[:, :], pbb[0][:, :], 1.0 / 256.0, msq[:, :], ALU.mult, ALU.subtract)
                P.act(var[:, :], var[:, :], AF.Sqrt, bias=EPS, scale=1.0)
                P.recip(var[:, :], var[:, :])
                for tix in (t0, t1):
                    P.tt("dve", T[4][:, :], hs[:, tix, :], mean[:, :], ALU.subtract)
                    P.stt("dve", T[4][:, :], T[4][:, :], cv[:, tix:tix + 1], var[:, :], ALU.mult, ALU.mult)
                    P.tt("dve", mixs[:, 8 + tix, :], T[4][:, :], hs[:, 8 + tix, :], ALU.mult)
            for dt in range(DT):
                b = wi % 2; wi += 1
                P.load(wtA[b][:, 0:32, :], wout[dt])
                for kt in range(32):
                    P.mm(pz[b][:, :], wtA[b][:, kt, :], mixs[:, kt, :], start=(kt == 0), stop=(kt == 31))
                P.tt("dve", xs[:, dt, :], xs[:, dt, :], pz[b][:, :], ALU.add)
            rmsnorm(xs, hs, 16)
            if moe:
                for sub in range(TB // 128):
                    ss = slice(sub * 128, (sub + 1) * 128)
                    for kt in range(KT):
                        P.mm(pr[:, 0:8], hs[:, kt, ss], rws[:, kt, :], start=(kt == 0), stop=(kt == KT - 1))
                    P.copy("act", lg[:, :], pr[:, 0:8])
                    P.op("dve", lambda e: e.reduce_max(mm_[:, 0:1], lg[:, :], mybir.AxisListType.X), ["lg"], ["mm_"])
                    P.ts("dve", eq1[:, :], lg[:, :], mm_[:, 0:1], None, ALU.is_equal)
                    P.stt("dve", l2[:, :], eq1[:, :], -1e30, lg[:, :], ALU.mult, ALU.add)
                    P.op("dve", lambda e: e.reduce_max(mm_[:, 1:2], l2[:, :], mybir.AxisListType.X), ["l2"], ["mm_"])
                    P.ts("dve", eq2[:, :], l2[:, :], mm_[:, 1:2], None, ALU.is_equal)
                    P.tt("dve", mm_[:, 2:3], mm_[:, 1:2], mm_[:, 0:1], ALU.subtract)
                    P.act(mm_[:, 3:4], mm_[:, 2:3], AF.Sigmoid)
                    P.act(mm_[:, 2:3], mm_[:, 2:3], AF.Sigmoid, scale=-1.0)
                    P.ts("dve", comb[:, :], eq1[:, :], mm_[:, 2:3], None, ALU.mult)
                    P.stt("dve", comb[:, :], eq2[:, :], mm_[:, 3:4], comb[:, :], ALU.mult, ALU.add)
                    for e_ in range(8):
                        P.ts("dve", rep[:, :], ones[:, :], comb[:, e_:e_ + 1], None, ALU.mult)
                        P.mm(pr[:, 0:128], rep[:, :], ident[:, :])
                        P.copy("act", cb[e_][:, ss], pr[:, 0:128])
            ag = mixs
            for g in range(NG):
                ex = (g * FG) // FPE if moe else 0
                for fi in range(FG):
                    ft = g * FG + fi
                    b = wi % 2; wi += 1
                    P.load(wtA[b][:, 0:KT, :], w1[ft])
                    P.load(wtB[b][:, :, :], w3[ft])
                    for kt in range(KT):
                        P.mm(pa[b][:, :], wtA[b][:, kt, :], hs[:, kt, :], start=(kt == 0), stop=(kt == KT - 1))
                    for kt in range(KT):
                        P.mm(pbb[b][:, :], wtB[b][:, kt, :], hs[:, kt, :], start=(kt == 0), stop=(kt == KT - 1))
                    P.act(sil[b][:, :], pa[b][:, :], AF.Silu)
                    P.tt("dve", ag[:, fi, :], sil[b][:, :], pbb[b][:, :], ALU.mult)
                    if moe:
                        P.tt("pool", ag[:, fi, :], ag[:, fi, :], cb[ex][:, :], ALU.mult)
                for dt in range(DT):
                    b = wi % 2; wi += 1
                    P.load(w2b[b][:, :, :], w2[g, dt])
                    for fi in range(FG):
                        P.mm(pz[b][:, :], w2b[b][:, fi, :], ag[:, fi, :], start=(fi == 0), stop=(fi == FG - 1))
                    P.tt("dve", xs[:, dt, :], xs[:, dt, :], pz[b][:, :], ALU.add)
            if last:
                rmsnorm(xs, hs, 16 + KT)
                P.store(outT[:, ts_].rearrange("(kt p) t -> p kt t", p=128), hs[:, 0:KT, :])
            else:
                P.store(outT[:, ts_].rearrange("(kt p) t -> p kt t", p=128), xs[:, :, :])
        P.emit()
    return P


def prep_C(inp, layer, cfg, xT, mixes, moe):
    f32 = np.float32
    D, FG = cfg.D, cfg.FG
    KT = D // 128
    TC = cfg.S // cfg.NC
    li = layer // 2
    mixall = np.stack(mixes)
    mixall = np.ascontiguousarray(mixall.transpose(1, 0, 2, 3)).reshape(40, 128, cfg.S)
    gluw = _tiles_lhsT(inp["s5_glu_w"][layer], 8)
    cvec = np.zeros((128, 16 + 2 * KT), f32)
    cvec[:, 0:8] = inp["s5_glu_b"][layer].reshape(8, 128).T
    cvec[:, 8:16] = inp["ret_norm_g"][layer].reshape(8, 128).T
    cvec[:, 16:16 + KT] = inp["norm_ffn_g"][layer].reshape(KT, 128).T
    cvec[:, 16 + KT:16 + 2 * KT] = inp["final_norm_g"].reshape(KT, 128).T
    wout = _tiles_lhsT(inp["w_out"][layer], 32)
    if moe:
        w1 = np.concatenate([_tiles_lhsT(inp["moe_w1"][li, e], KT) for e in range(cfg.NE)])
        w3 = np.concatenate([_tiles_lhsT(inp["moe_w3"][li, e], KT) for e in range(cfg.NE)])
        w2full = inp["moe_w2"][li].reshape(cfg.NE * cfg.DFE, D)
    else:
        w1 = _tiles_lhsT(inp["ffn_w1"][li], KT)
        w3 = _tiles_lhsT(inp["ffn_w3"][li], KT)
        w2full = inp["ffn_w2"][li]
    NF = w2full.shape[0] // 128
    NG = NF // FG
    w2 = np.ascontiguousarray(w2full.reshape(NG, FG, 128, KT, 128).transpose(0, 3, 2, 1, 4))
    ones = np.ones((128, 128), f32)
    ident = np.eye(128, dtype=f32)
    maps = []
    for c in range(cfg.NC):
        tsl = slice(c * TC, (c + 1) * TC)
        m = dict(xT=np.ascontiguousarray(xT[:, tsl]), mixin=np.ascontiguousarray(mixall[:, :, tsl]),
                 gluw=gluw, cvec=cvec, wout=wout, w1=w1, w3=w3, w2=w2, ones=ones, ident=ident)
        if moe:
            m["rw"] = np.ascontiguousarray(inp["router_w"][li].reshape(KT, 128, 8).transpose(1, 0, 2))
        maps.append(m)
    return maps


_PROGS = {}


def _prog(key, fn):
    if key not in _PROGS:
        _PROGS[key] = fn()
    return _PROGS[key]


def run_model(inp, cfg, depth=2):
    x = np.asarray(inp["x"], np.float32)
    xT = np.ascontiguousarray(x[0].T)
    cores = list(range(8))
    for layer in range(depth):
        moe = (layer % 2 == 1)
        last = (layer == depth - 1)
        PB = _prog(("B", cfg.D, cfg.S), lambda: build_B(cfg))
        res = run_bass_kernel_spmd(PB.nc, prep_B(inp, layer, cfg, xT), core_ids=cores)
        mixes = [r["mix"] for r in res.results]
        PC = _prog(("C", cfg.D, cfg.S, moe, last), lambda: build_C(cfg, moe, last))
        res = run_bass_kernel_spmd(PC.nc, prep_C(inp, layer, cfg, xT, mixes, moe), core_ids=list(range(cfg.NC)))
        xT = np.concatenate([r["outT"] for r in res.results], axis=1)
    return np.ascontiguousarray(xT.T)[None].astype(np.float32)


def kernel(**inputs):
    inp = {k: np.asarray(v) for k, v in inputs.items()}
    return run_model(inp, Cfg())
```
